# Optimizing a Trainium2 kernel written in Bass

```python
import math
import jax, jax.numpy as jnp
from jax import lax
import numpy as np

D_MODEL = 2048
BATCH = 4
SEQ = 2048
DEPTH = 4

N_BRANCHES = 3
POOL_WINDOWS = (2, 4, 8, 16)
N_POOL_GROUPS = 4
POOL_DIM = D_MODEL // 2
POOL_GROUP_DIM = POOL_DIM // N_POOL_GROUPS
CONV_DIM = D_MODEL // 2
CONV_WIDTH = 3
N_HEADS = 8
HEAD_DIM = 128
N_KV_HEADS = 2
GROUP = N_HEADS // N_KV_HEADS
ATTN_DIM = N_HEADS * HEAD_DIM
IDX_HEADS = 16
IDX_DIM = 64
TOPK_MAX = 256
Q_BLOCK = 128
NUM_BUCKETS = 32
MAX_EXACT = NUM_BUCKETS // 2
MAX_DISTANCE = 128
BRANCH_DIM = D_MODEL // 2
MLP_HIDDEN = 4 * D_MODEL
NORM_EPS = 1e-6
IN_SPLITS = (POOL_DIM, CONV_DIM, CONV_DIM, CONV_DIM, ATTN_DIM, N_KV_HEADS * HEAD_DIM,
             N_KV_HEADS * HEAD_DIM, IDX_HEADS * IDX_DIM, IDX_DIM, IDX_HEADS)
IN_WIDTH = sum(IN_SPLITS)

kernel_name = 'hybrid_gated_pool_conv_dsa_block'


def _split_points():
    pts, acc = [], 0
    for w in IN_SPLITS[:-1]:
        acc += w
        pts.append(acc)
    return pts


def rmsnorm(x, g):
    xf = x.astype(jnp.float32)
    y = xf * lax.rsqrt(jnp.mean(jnp.square(xf), axis=-1, keepdims=True) + NORM_EPS)
    return (y * g.astype(jnp.float32)).astype(x.dtype)


def t5_bucket(dist):
    n = jnp.maximum(dist, 0)
    nf = jnp.maximum(n, 1).astype(jnp.float32)
    large = MAX_EXACT + (jnp.log(nf / MAX_EXACT) / math.log(MAX_DISTANCE / MAX_EXACT)
                         * (NUM_BUCKETS - MAX_EXACT)).astype(jnp.int32)
    large = jnp.minimum(large, NUM_BUCKETS - 1)
    return jnp.where(n < MAX_EXACT, n, large)


def pool_mixer(u, pool_w, pool_scale):
    B, S, _ = u.shape
    ug = u.astype(jnp.float32).reshape(B, S, N_POOL_GROUPS, POOL_GROUP_DIM)
    c0 = jnp.concatenate([jnp.zeros((B, 1, N_POOL_GROUPS, POOL_GROUP_DIM), jnp.float32),
                          jnp.cumsum(ug, axis=1)], axis=1)
    t = jnp.arange(S)
    win = jnp.array(POOL_WINDOWS, jnp.int32)
    lo = jnp.maximum(t[:, None] + 1 - win[None, :], 0)
    g_ar = jnp.arange(N_POOL_GROUPS)[None, :]
    window_sum = c0[:, 1:] - c0[:, lo, g_ar]
    cnt = (t[:, None] + 1 - lo).astype(jnp.float32)[None, :, :, None]
    d = window_sum / cnt - ug
    y = jnp.einsum('bsgc,gcd->bsgd', d, pool_w.astype(jnp.float32))
    return (y.reshape(B, S, POOL_DIM) * pool_scale.astype(jnp.float32)).astype(u.dtype)


def conv_mixer(u, gate_c, gate_b, conv_w):
    z = gate_c * u
    y = lax.conv_general_dilated(z, conv_w[:, None, :], window_strides=(1,),
                                 padding=[(CONV_WIDTH - 1, 0)],
                                 dimension_numbers=('NWC', 'WIO', 'NWC'),
                                 feature_group_count=CONV_DIM)
    return gate_b * y


def sparse_attention(q, k, v, q_idx, k_idx, w_idx, rel_bias):
    B, S = q.shape[0], q.shape[1]
    topk = min(TOPK_MAX, S // 4)
    nblk = S // Q_BLOCK
    key_pos = jnp.arange(S)
    k_idx32 = k_idx.astype(jnp.float32)
    table = rel_bias.astype(jnp.float32)

    def to_blocks(a):
        return jnp.moveaxis(a.reshape((B, nblk, Q_BLOCK) + a.shape[2:]), 1, 0)

    def block_fn(args):
        qb, qib, wb, blk = args
        q_pos = blk * Q_BLOCK + jnp.arange(Q_BLOCK)
        dots = jnp.einsum('bthd,bsd->bths', qib.astype(jnp.float32), k_idx32)
        score = jnp.einsum('bth,bths->bts', wb.astype(jnp.float32) * IDX_HEADS ** -0.5,
                           jax.nn.relu(dots))
        causal = key_pos[None, :] <= q_pos[:, None]
        score = jnp.where(causal[None], score, -jnp.inf)
        _, idx = lax.top_k(score, topk)
        k_sel = jax.vmap(lambda kb, ib: kb[ib])(k, idx)
        v_sel = jax.vmap(lambda vb, ib: vb[ib])(v, idx)
        qg = qb.reshape(B, Q_BLOCK, N_KV_HEADS, GROUP, HEAD_DIM).astype(jnp.float32)
        logits = jnp.einsum('btgrd,btkgd->btgrk', qg, k_sel.astype(jnp.float32)) * HEAD_DIM ** -0.5
        dist = q_pos[None, :, None] - idx
        bias = table[t5_bucket(dist)]
        bias = bias.reshape(B, Q_BLOCK, topk, N_KV_HEADS, GROUP).transpose(0, 1, 3, 4, 2)
        logits = jnp.where((dist >= 0)[:, :, None, None, :], logits + bias, -jnp.inf)
        p = jax.nn.softmax(logits, axis=-1)
        o = jnp.einsum('btgrk,btkgd->btgrd', p, v_sel.astype(jnp.float32))
        return o.reshape(B, Q_BLOCK, ATTN_DIM).astype(q.dtype)

    out = lax.map(block_fn, (to_blocks(q), to_blocks(q_idx), to_blocks(w_idx),
                             jnp.arange(nblk, dtype=jnp.int32)))
    return jnp.moveaxis(out, 0, 1).reshape(B, S, ATTN_DIM)


def setup_inputs(seed: int = 0) -> dict:
    key = jax.random.key(seed)
    ks = jax.random.split(key, 13)

    def nrm(k, shape, scale):
        return jax.random.normal(k, shape, jnp.float32) * scale

    return {
        'x': nrm(ks[0], (BATCH, SEQ, D_MODEL), 1.0),
        'norm_gains': 1.0 + nrm(ks[1], (DEPTH, 4, D_MODEL), 0.05),
        'w_in': nrm(ks[2], (DEPTH, D_MODEL, IN_WIDTH), D_MODEL ** -0.5),
        'pool_w': nrm(ks[3], (DEPTH, N_POOL_GROUPS, POOL_GROUP_DIM, POOL_GROUP_DIM), POOL_GROUP_DIM ** -0.5),
        'pool_scale': 1.0 + nrm(ks[4], (DEPTH, POOL_DIM), 0.05),
        'conv_w': nrm(ks[5], (DEPTH, CONV_WIDTH, CONV_DIM), CONV_WIDTH ** -0.5),
        'rel_bias': nrm(ks[6], (NUM_BUCKETS, N_HEADS), 0.5),
        'w_branch': nrm(ks[7], (DEPTH, N_BRANCHES, BRANCH_DIM, D_MODEL), BRANCH_DIM ** -0.5),
        'w_gate': nrm(ks[8], (DEPTH, D_MODEL, N_BRANCHES * D_MODEL), D_MODEL ** -0.5),
        'b_gate': nrm(ks[9], (DEPTH, N_BRANCHES, D_MODEL), 0.1),
        'w_out': nrm(ks[10], (DEPTH, D_MODEL, D_MODEL), D_MODEL ** -0.5),
        'w_up': nrm(ks[11], (DEPTH, D_MODEL, MLP_HIDDEN), D_MODEL ** -0.5),
        'w_down': nrm(ks[12], (DEPTH, MLP_HIDDEN, D_MODEL), MLP_HIDDEN ** -0.5),
    }


def reference(x, norm_gains, w_in, pool_w, pool_scale, conv_w, rel_bias, w_branch,
              w_gate, b_gate, w_out, w_up, w_down):
    B, S, D = x.shape
    pts = _split_points()
    for l in range(DEPTH):
        g = norm_gains[l]
        h = rmsnorm(x, g[0])
        proj = h @ w_in[l]
        (u_pool, u_conv, c_gate, b_gate_conv, q, k, v, qi, ki, wi) = jnp.split(proj, pts, axis=-1)
        y_pool = pool_mixer(u_pool, pool_w[l], pool_scale[l])
        y_conv = conv_mixer(u_conv, c_gate, b_gate_conv, conv_w[l])
        y_attn = sparse_attention(q.reshape(B, S, N_HEADS, HEAD_DIM),
                                  k.reshape(B, S, N_KV_HEADS, HEAD_DIM),
                                  v.reshape(B, S, N_KV_HEADS, HEAD_DIM),
                                  qi.reshape(B, S, IDX_HEADS, IDX_DIM), ki, wi, rel_bias)
        branches = jnp.stack([y_pool, y_conv, y_attn], axis=2)
        up = jnp.einsum('bsnc,ncd->bsnd', branches, w_branch[l])
        gates = jax.nn.sigmoid((h @ w_gate[l]).reshape(B, S, N_BRANCHES, D) + b_gate[l])
        mixed = jnp.sum(gates * up, axis=2) @ w_out[l]
        x = x + rmsnorm(mixed, g[1])
        h = rmsnorm(x, g[2])
        m = jnp.square(jax.nn.relu(h @ w_up[l])) @ w_down[l]
        x = x + rmsnorm(m, g[3])
    return x
```

```python
import math
import contextlib
import numpy as np
import ml_dtypes
import concourse.bass as bass
import concourse.mybir as mybir
from concourse.bass_utils import run_bass_kernel_spmd

F32 = mybir.dt.float32
BF16 = mybir.dt.bfloat16
AF = mybir.ActivationFunctionType
ALU = mybir.AluOpType

D = 2048
NT = 1024
SEQ = 2048
NB = 16
HID = 8192
INW = 6736
CHUNKS = ((0, 3, 4, 7), (1, 2, 5, 6))
WINS = (2, 4, 8, 16)
EPS = 1e-6
NEG_MASK = -3.0e38
NEG_REPL = -1.0e30


class Dummy:
    def __getitem__(self, k):
        return self

    def __getattr__(self, k):
        return self

    def __call__(self, *a, **k):
        return self


class Tok:
    __slots__ = ("w", "r")

    def __init__(self):
        self.w = None
        self.r = []


class Eng:
    def __init__(self, name, handle, sem):
        self.name = name
        self.h = handle
        self.sem = sem
        self.count = 0
        self.waited = {}


class Ctx:
    def __init__(self, nc):
        self.nc = nc
        self.plan = True
        self.stack = contextlib.ExitStack()

    def setup(self):
        nc = self.nc
        st = self.stack
        self.eng = {}
        for name, h in (("pe", nc.tensor), ("act", nc.scalar), ("dve", nc.vector),
                        ("pool", nc.gpsimd), ("sp", nc.sync)):
            sem = st.enter_context(nc.semaphore("s_" + name))
            self.eng[name] = Eng(name, h, sem)
        self.dsems = [st.enter_context(nc.semaphore("d%d" % i)) for i in range(40)]
        self.dcount = [0] * len(self.dsems)
        self.dnext = 0
        self.ccsem = st.enter_context(nc.semaphore("cc"))
        self.cccount = 0

    @contextlib.contextmanager
    def sbuf(self, name, shape, dtype):
        if self.plan:
            yield Dummy()
        else:
            self.uid = getattr(self, "uid", 0) + 1
            with self.nc.sbuf_tensor("sb%d_%s" % (self.uid, name), shape, dtype) as t:
                yield t

    def _waits(self, E, reads, writes):
        need = {}

        def add(ev):
            if ev is None:
                return
            sem, val = ev
            k = id(sem)
            if k not in need or need[k][1] < val:
                need[k] = (sem, val)
        for t in reads:
            add(t.w)
        for t in writes:
            add(t.w)
            for ev in t.r:
                add(ev)
        for k, (sem, val) in need.items():
            if E.name == "pe" and sem is E.sem:
                continue
            if E.waited.get(k, 0) < val:
                E.h.wait_ge(sem, val)
                E.waited[k] = val

    def _record(self, ev, reads, writes):
        for t in reads:
            t.r.append(ev)
            if len(t.r) > 64:
                t.r = t.r[-64:] if False else t.r
        for t in writes:
            t.w = ev
            t.r = []

    def op(self, en, emit, reads=(), writes=(), inc=True):
        if self.plan:
            return
        E = self.eng[en]
        self._waits(E, reads, writes)
        ins = emit(E.h)
        if inc:
            E.count += 1
            ins.then_inc(E.sem, 1)
            ev = (E.sem, E.count)
        else:
            ev = (E.sem, E.count + 1)
        self._record(ev, reads, writes)

    def dma(self, qn, out, in_, reads=(), writes=()):
        if self.plan:
            return
        E = self.eng[qn]
        self._waits(E, reads, writes)
        i = self.dnext
        self.dnext = (self.dnext + 1) % len(self.dsems)
        sem = self.dsems[i]
        k = id(sem)
        if self.dcount[i] > 0 and E.waited.get(k, 0) < self.dcount[i]:
            E.h.wait_ge(sem, self.dcount[i])
            E.waited[k] = self.dcount[i]
        E.h.dma_start(out=out, in_=in_).then_inc(sem, 16)
        self.dcount[i] += 16
        ev = (sem, self.dcount[i])
        self._record(ev, reads, writes)

    def allgather(self, in_t, out_t, reads=(), writes=()):
        if self.plan:
            return
        E = self.eng["pool"]
        self._waits(E, reads, writes)
        E.h.collective_compute("AllGather", ALU.bypass,
                               replica_groups=[[0, 1], [2, 3], [4, 5], [6, 7]],
                               ins=[in_t.ap().opt()], outs=[out_t.ap().opt()]).then_inc(self.ccsem)
        self.cccount += 1
        ev = (self.ccsem, self.cccount)
        self._record(ev, reads, writes)

    def barrier(self, engines=("pe", "act", "dve", "sp", "pool")):
        if self.plan:
            return
        evs = []
        for n in ("pe", "act", "dve", "pool"):
            E = self.eng[n]
            if E.count > 0:
                evs.append((E.sem, E.count))
        for i, sem in enumerate(self.dsems):
            if self.dcount[i] > 0:
                evs.append((sem, self.dcount[i]))
        for n in engines:
            E = self.eng[n]
            for sem, val in evs:
                if sem is E.sem:
                    continue
                k = id(sem)
                if E.waited.get(k, 0) < val:
                    E.h.wait_ge(sem, val)
                    E.waited[k] = val


class WStream:
    def __init__(self, cx, nbuf, elems):
        self.cx = cx
        self.nbuf = nbuf
        self.elems = elems
        self.planlist = []
        self.bufs = None
        self.toks = [Tok() for _ in range(nbuf)]
        self.reset()

    def reset(self):
        self.next_get = 0
        self.next_issue = 0

    def get(self, tag, dmas):
        cx = self.cx
        if cx.plan:
            self.planlist.append((tag, dmas))
            return Dummy(), Tok()
        i = self.next_get
        assert self.planlist[i][0] == tag, (self.planlist[i][0], tag)
        lim = min(len(self.planlist), i + self.nbuf)
        while self.next_issue < lim:
            j = self.next_issue
            buf = self.bufs[j % self.nbuf]
            tok = self.toks[j % self.nbuf]
            for dst_fn, src in self.planlist[j][1]:
                cx.dma("pool", dst_fn(buf), src, writes=[tok])
            self.next_issue += 1
        self.next_get += 1
        return self.bufs[i % self.nbuf], self.toks[i % self.nbuf]


def build(L, debug=False):
    nc = bass.Bass("TRN2", target_bir_lowering=False)
    dt0 = nc.dram_tensor

    def dt(name, shape, dtype, kind=None):
        if kind is not None:
            return dt0(name, shape, dtype, kind=kind)
        if debug and name.startswith(("s_", "xTs")):
            return dt0(name, shape, dtype, kind="ExternalOutput")
        return dt0(name, shape, dtype)

    def ext(name, shape, dtype=F32):
        return dt(name, list(shape), dtype, kind="ExternalInput").ap()

    xin = ext("xT", [D, NT])
    w_in = ext("w_in", [L, D, INW])
    pool_w = ext("pool_w", [L, 4, 256, 256])
    w_branch = ext("w_branch", [L, 3, 1024, D])
    w_gate = ext("w_gate", [L, D, 3 * D])
    w_out = ext("w_out", [L, D, D])
    w_up = ext("w_up", [L, D, HID])
    w_down = ext("w_down", [L, HID, D])
    gains_in = ext("gains", [128, L * 4 * NB])
    pscale_in = ext("pscale", [128, L * 8])
    convw_in = ext("convw", [128, L * 3 * 8])
    bgate_in = ext("bgate", [128, L * 3 * NB])
    tb_in = ext("tb", [128, 3 * 8 * 128])
    qpos_in = ext("qpos", [128, 8])
    kpos_in = ext("kpos", [128, SEQ], mybir.dt.uint16)
    sel_in = ext("sel", [128, 8 * 16 * 2])
    selh_in = ext("selh", [128, 4 * 4 * 16])
    invc_in = ext("invc", [128, 4 * 4 * 16])
    ident_in = ext("ident", [128, 128], BF16)
    yout = dt("yT", [D, NT], F32, kind="ExternalOutput").ap()

    xT = dt("xTs", [D, NT], F32)
    s_upool = dt("s_upool", [1024, NT], BF16)
    s_z = dt("s_z", [1024, NT], BF16)
    s_bconv = dt("s_bconv", [1024, NT], BF16)
    s_q = dt("s_q", [1024, NT], BF16)
    s_qi = dt("s_qi", [1024, NT], BF16)
    s_ypool = dt("s_ypool", [1024, NT], BF16)
    s_yconv = dt("s_yconv", [1024, NT], BF16)
    s_yattn = dt("s_yattn", [1024, NT], BF16)
    xk_in = dt("xk_in", [576, NT], BF16)
    xk_out = dt("xk_out", [1152, NT], BF16)
    xh_in = dt("xh_in", [2048, 64], BF16)
    xh_out = dt("xh_out", [4096, 64], BF16)
    s_dbgmask = dt("s_dbgmask", [1024, SEQ], BF16) if debug else None
    s_dbgsc = dt("s_dbgsc", [1024, SEQ], F32) if debug else None
    s_dbgK = dt("s_dbgK", [128, 2 * SEQ], BF16) if debug else None
    s_dbgV = dt("s_dbgV", [128, 16 * 2 * 128], BF16) if debug else None
    s_dbgX = dt("s_dbgX", [1152, NT], BF16) if debug else None
    s_dbglg = dt("s_dbglg", [8 * 2 * 16 * 128, 512], F32) if debug else None
    s_dbgpp = dt("s_dbgpp", [8 * 2 * 16 * 128, 512], BF16) if debug else None
    s_dbgod = dt("s_dbgod", [8 * 2 * 2 * 128, 512], F32) if debug else None
    TX = [Tok() for _ in range(NB)]
    T = {n: Tok() for n in ( "upool", "z", "bconv", "q", "qi", "ypool", "yconv", "yattn",
                            "xk_in", "xk_out", "xh_in", "xh_out", "yout")}

    cx = Ctx(nc)

    def emit_all():
        op = cx.op
        dma = cx.dma
        with contextlib.ExitStack() as top:
            def SB(name, shape, dtype):
                return top.enter_context(cx.sbuf(name, shape, dtype))
            hT = SB("hT", [128, NB, NT], BF16)
            slabs = [SB("slab%d" % i, [128, 16 * 512], BF16) for i in range(3)]
            ws.bufs = slabs
            gains = SB("gains", [128, L * 4 * NB], F32)
            pscale = SB("pscale", [128, L * 8], F32)
            convw = SB("convw", [128, L * 3 * 8], F32)
            bgate = SB("bgate", [128, L * 3 * NB], F32)
            qpos = SB("qpos", [128, 8], F32)
            sel = SB("sel", [128, 8, 16, 2], F32)
            selh = SB("selh", [128, 4, 4, 16], F32)
            invc = SB("invc", [128, 4, 4, 16], F32)
            ident = SB("ident", [128, 128], BF16)
            ones = SB("ones", [128, 128], BF16)
            negc = SB("negc", [128, 1], F32)
            wi_sb = SB("wi_sb", [128, 8, 16], F32)
            pw = SB("pw", [128, 4, 2, 256], BF16)
            t_pw = Tok()
            if cx.plan:
                ps = Dummy()
                psb = Dummy()
            else:
                ps = top.enter_context(nc.psum_tensor("ps", [128, 7, 512], F32))
                psb = top.enter_context(nc.psum_tensor("psb", [128, 1, 1024], BF16))
            pst = [Tok() for _ in range(7)]
            psbt = [Tok() for _ in range(2)]
            t_hT = [Tok(), Tok()]
            t_const = Tok()
            t_wi = Tok()

            for dst, src in ((gains, gains_in), (pscale, pscale_in), (convw, convw_in),
                             (bgate, bgate_in), (qpos, qpos_in), (ident, ident_in)):
                dma("sp", dst[:], src[:, :], writes=[t_const])
            dma("sp", sel[:], sel_in.rearrange("p (a b c) -> p a b c", a=8, b=16), writes=[t_const])
            dma("sp", selh[:], selh_in.rearrange("p (a b c) -> p a b c", a=4, b=4), writes=[t_const])
            dma("sp", invc[:], invc_in.rearrange("p (a b c) -> p a b c", a=4, b=4), writes=[t_const])
            op("dve", lambda e: e.memset(ones[:], 1.0), writes=[t_const])
            op("dve", lambda e: e.memset(negc[:], -30000.0), writes=[t_const])
            dma("sp", xT.ap()[:, :], xin[:, :], writes=TX)

            bank_rr = [0]

            def bank():
                b = bank_rr[0]
                bank_rr[0] = (b + 1) % 7
                return b

            def gcol(l, i, kb):
                return gains[:, (l * 4 + i) * NB + kb:(l * 4 + i) * NB + kb + 1]

            def norm_to_h(l, gi, tts=(0, 1)):
                with cx.sbuf("nx", [128, 2, NB, 512], F32) as nx2, \
                        cx.sbuf("nsq", [128, 2, NB, 512], BF16) as nsq2, \
                        cx.sbuf("nr", [128, 2, 512], F32) as nr2:
                    t_nx2, t_sq2, t_r2 = [Tok(), Tok()], [Tok(), Tok()], [Tok(), Tok()]
                    for tt in tts:
                        tsl = slice(tt * 512, (tt + 1) * 512)
                        dma("sp", nx2[:, tt], xT.ap().rearrange("(kb p) t -> p kb t", p=128)[:, :, tsl],
                            reads=TX, writes=[t_nx2[tt]])
                    for tt in tts:
                        op("act", lambda e, tt=tt: e.activation(out=nsq2[:, tt], in_=nx2[:, tt], func=AF.Square),
                           reads=[t_nx2[tt]], writes=[t_sq2[tt]])
                    for tt in tts:
                        tsl = slice(tt * 512, (tt + 1) * 512)
                        nx, nsq, nr = nx2[:, tt], nsq2[:, tt], nr2[:, tt]
                        t_nx, t_sq, t_r = t_nx2[tt], t_sq2[tt], t_r2[tt]
                        b = bank()
                        for kb in range(NB):
                            op("pe", lambda e, kb=kb, b=b: e.matmul(ps[:, b, :], ones[:], nsq[:, kb, :],
                                                                   start=(kb == 0), stop=(kb == NB - 1)),
                               reads=[t_sq, t_const], writes=[pst[b]], inc=(kb == NB - 1))
                        op("act", lambda e, b=b: e.activation(out=nr, in_=ps[:, b, :], func=AF.Sqrt,
                                                              bias=EPS, scale=1.0 / D),
                           reads=[pst[b]], writes=[t_r])
                        op("dve", lambda e: e.reciprocal(out=nr, in_=nr), reads=[t_r], writes=[t_r])
                        for kb in range(NB):
                            op("dve", lambda e, kb=kb: e.scalar_tensor_tensor(
                                out=hT[:, kb, tsl], in0=nx[:, kb, :], scalar=gcol(l, gi, kb), in1=nr,
                                op0=ALU.mult, op1=ALU.mult),
                               reads=[t_nx, t_r, t_const], writes=[t_hT[tt]])
                cx.barrier()

            def gemm_block(wbuf_view, wtok, nkb, rhs_fn, rhs_toks, col0, ncols=128):
                b = bank()
                for kb in range(nkb):
                    op("pe", lambda e, kb=kb, b=b: e.matmul(ps[0:ncols, b, :], wbuf_view[:, kb, col0:col0 + ncols],
                                                           rhs_fn(kb), start=(kb == 0), stop=(kb == nkb - 1)),
                       reads=[wtok] + rhs_toks, writes=[pst[b]], inc=(kb == nkb - 1))
                return b

            def win_slab(l, c0, ncols):
                def dst(buf):
                    return buf[:, 0:16 * ncols].rearrange("p (kb n) -> p kb n", kb=16)
                src = w_in[l].rearrange("(kb p) n -> p kb n", p=128)[:, :, c0:c0 + ncols]
                return [(dst, src)]

            def in_proj(l):
                with contextlib.ExitStack() as es:
                    def A(name, shape, dtype):
                        return es.enter_context(cx.sbuf(name, shape, dtype))
                    stg = A("stg", [128, 4, NT], BF16)
                    ustg = A("ustg", [128, 4, NT], F32)
                    ue = A("ue", [128, 8, 4, 272], BF16)
                    tl = A("tl", [128, 8, 2, 4, 16], BF16)
                    hf = A("hf", [128, 4, 16], F32)
                    htmp = A("htmp", [128, 4, 16], F32)
                    a1 = A("a1", [128, 4, 272], F32)
                    a2 = A("a2", [128, 4, 272], F32)
                    dd = A("dd", [128, 4, NT], BF16)
                    dfix = A("dfix", [128, 4, 16], F32)
                    acc = A("acc", [128, 4, 256], F32)
                    bcv = A("bcv", [128, 8, NT], BF16)
                    ost = A("ost", [128, 4, NT], BF16)
                    t_stg = [Tok() for _ in range(4)]
                    t_u = [Tok() for _ in range(4)]
                    stg_rr = [0]
                    t_ue = [Tok() for _ in range(8)]
                    t_tl = [Tok() for _ in range(8)]
                    t_hf, t_a1, t_a2, t_acc, t_dfix = Tok(), Tok(), Tok(), Tok(), Tok()
                    t_dd = [Tok() for _ in range(4)]
                    t_bcv = [Tok() for _ in range(8)]
                    t_ost = [Tok() for _ in range(4)]

                    def hrhs(tt):
                        return lambda kb: hT[:, kb, tt * 512:(tt + 1) * 512]

                    def plain(tag, c0, nblk, dst_t, dst_tok, row0, scale=1.0, tails=None):
                        buf, wtok = ws.get(tag, win_slab(l, c0, nblk * 128))
                        wv = buf[:, 0:16 * nblk * 128].rearrange("p (kb n) -> p kb n", kb=16)
                        for cb in range(nblk):
                            si = stg_rr[0]
                            stg_rr[0] = (si + 1) % 4
                            for tt in range(2):
                                b = gemm_block(wv, wtok, 16, hrhs(tt), [t_hT[tt]], cb * 128)
                                op("act", lambda e, b=b, si=si, tt=tt: e.activation(
                                    out=stg[:, si, tt * 512:(tt + 1) * 512], in_=ps[:, b, :], func=AF.Copy,
                                    scale=scale), reads=[pst[b]], writes=[t_stg[si]])
                            r0 = row0 + cb * 128
                            dma("sp", dst_t.ap()[r0:r0 + 128, :], stg[:, si, :], reads=[t_stg[si]], writes=[dst_tok])
                            if tails is not None:
                                tr0 = tails + cb * 128
                                dma("sp", xh_in.ap()[tr0:tr0 + 128, :].rearrange("p (j w) -> p j w", j=4),
                                    stg[:, si, :].rearrange("p (j w) -> p j w", j=4)[:, :, 240:256],
                                    reads=[t_stg[si]], writes=[T["xh_in"]])
                            yield

                    def drain(g):
                        for _ in g:
                            pass

                    drain(plain("pu0_%d" % l, 0, 4, s_upool, T["upool"], 0, tails=0))
                    drain(plain("pu1_%d" % l, 512, 4, s_upool, T["upool"], 512, tails=512))
                    for half in range(2):
                        buf, wtok = ws.get("cu%d_%d" % (half, l), win_slab(l, 1024 + half * 512, 512))
                        wv = buf[:, :].rearrange("p (kb n) -> p kb n", kb=16)
                        for cb in range(4):
                            for tt in range(2):
                                b = gemm_block(wv, wtok, 16, hrhs(tt), [t_hT[tt]], cb * 128)
                                op("act", lambda e, b=b, cb=cb, tt=tt: e.activation(
                                    out=ustg[:, cb, tt * 512:(tt + 1) * 512], in_=ps[:, b, :], func=AF.Copy),
                                   reads=[pst[b]], writes=[t_u[cb]])
                        buf, wtok = ws.get("cc%d_%d" % (half, l), win_slab(l, 2048 + half * 512, 512))
                        wv = buf[:, :].rearrange("p (kb n) -> p kb n", kb=16)
                        for cb in range(4):
                            si = stg_rr[0]
                            stg_rr[0] = (si + 1) % 4
                            for tt in range(2):
                                b = gemm_block(wv, wtok, 16, hrhs(tt), [t_hT[tt]], cb * 128)
                                op("dve", lambda e, b=b, cb=cb, tt=tt, si=si: e.tensor_tensor(
                                    out=stg[:, si, tt * 512:(tt + 1) * 512], in0=ps[:, b, :],
                                    in1=ustg[:, cb, tt * 512:(tt + 1) * 512], op=ALU.mult),
                                   reads=[pst[b], t_u[cb]], writes=[t_stg[si]])
                            r0 = half * 512 + cb * 128
                            dma("sp", s_z.ap()[r0:r0 + 128, :], stg[:, si, :], reads=[t_stg[si]], writes=[T["z"]])
                            dma("sp", xh_in.ap()[1024 + r0:1024 + r0 + 128, :].rearrange("p (j w) -> p j w", j=4),
                                stg[:, si, :].rearrange("p (j w) -> p j w", j=4)[:, :, 240:256],
                                reads=[t_stg[si]], writes=[T["xh_in"]])
                    cx.allgather(xh_in, xh_out, reads=[T["xh_in"]], writes=[T["xh_out"]])

                    def g_gemm():
                        yield from plain("kv%d" % l, 5120, 4, xk_in, T["xk_in"], 0)
                        buf, wtok = ws.get("kiwi%d" % l, win_slab(l, 6656, 80))
                        wv = buf[:, 0:16 * 80].rearrange("p (kb n) -> p kb n", kb=16)
                        si = stg_rr[0]
                        stg_rr[0] = (si + 1) % 4
                        for tt in range(2):
                            b = gemm_block(wv, wtok, 16, hrhs(tt), [t_hT[tt]], 0, ncols=64)
                            op("act", lambda e, b=b, si=si, tt=tt: e.activation(
                                out=stg[0:64, si, tt * 512:(tt + 1) * 512], in_=ps[0:64, b, :], func=AF.Copy),
                               reads=[pst[b]], writes=[t_stg[si]])
                        dma("sp", xk_in.ap()[512:576, :], stg[0:64, si, :], reads=[t_stg[si]], writes=[T["xk_in"]])
                        b = bank()
                        for qb in range(8):
                            for kb in range(NB):
                                op("pe", lambda e, kb=kb, qb=qb, b=b: e.matmul(
                                    ps[:, b, qb * 16:(qb + 1) * 16], hT[:, kb, qb * 128:(qb + 1) * 128],
                                    wv[:, kb, 64:80], start=(kb == 0), stop=(kb == NB - 1)),
                                   reads=[wtok, t_hT[qb // 4]], writes=[pst[b]], inc=(kb == NB - 1))
                        op("act", lambda e, b=b: e.activation(out=wi_sb[:].rearrange("p a b -> p (a b)"),
                                                              in_=ps[:, b, 0:128], func=AF.Copy, scale=0.25),
                           reads=[pst[b]], writes=[t_wi])
                        cx.allgather(xk_in, xk_out, reads=[T["xk_in"]], writes=[T["xk_out"]])
                        yield
                        yield from plain("cb0_%d" % l, 3072, 4, s_bconv, T["bconv"], 0)
                        yield from plain("cb1_%d" % l, 3584, 4, s_bconv, T["bconv"], 512)
                        yield from plain("q0_%d" % l, 4096, 4, s_q, T["q"], 0, scale=128 ** -0.5)
                        yield from plain("q1_%d" % l, 4608, 4, s_q, T["q"], 512, scale=128 ** -0.5)
                        yield from plain("qi0_%d" % l, 5632, 4, s_qi, T["qi"], 0)
                        yield from plain("qi1_%d" % l, 6144, 4, s_qi, T["qi"], 512)

                    def load_blk(src_t, src_tok, trow0, blk):
                        r0 = blk * 128
                        dma("sp", ue[:, blk, :, 16:272], src_t.ap()[r0:r0 + 128, :].rearrange("p (j w) -> p j w", j=4),
                            reads=[src_tok], writes=[t_ue[blk]])
                        for r in range(2):
                            rr = r * 2048 + trow0 + r0
                            dma("sp", tl[:, blk, r], xh_out.ap()[rr:rr + 128, :].rearrange("p (j w) -> p j w", j=4),
                                reads=[T["xh_out"]], writes=[t_tl[blk]])

                    def halo_blk(blk):
                        si = blk
                        cands = ((0, -1), (1, 0), (1, -1), (0, 0))
                        op("dve", lambda e: e.memset(hf[:], 0.0), writes=[t_hf])
                        for ci, (r, dj) in enumerate(cands):
                            j0 = 1 if dj == -1 else 0
                            srcv = tl[:, si, r, j0 + dj:4 + dj, :]
                            selv = selh[:, ci, j0:4, :]
                            op("dve", lambda e, srcv=srcv, selv=selv, j0=j0: e.tensor_tensor(
                                out=htmp[:, j0:4, :], in0=srcv, in1=selv, op=ALU.mult),
                               reads=[t_tl[si], t_const, t_hf], writes=[t_hf])
                            op("dve", lambda e, j0=j0: e.tensor_tensor(
                                out=hf[:, j0:4, :], in0=hf[:, j0:4, :], in1=htmp[:, j0:4, :], op=ALU.add),
                               reads=[t_hf], writes=[t_hf])
                        op("dve", lambda e, si=si: e.tensor_copy(out=ue[:, si, :, 0:16], in_=hf[:]),
                           reads=[t_hf, t_ue[si]], writes=[t_ue[si]])

                    def pool_mm(g):
                        for ob in range(2):
                            osl = (g % 2) * 2 + ob
                            for tt in range(2):
                                b = bank()
                                for cb in range(2):
                                    dsl = (g % 2) * 2 + cb
                                    op("pe", lambda e, b=b, cb=cb, ob=ob, tt=tt, dsl=dsl: e.matmul(
                                        ps[:, b, :], pw[:, g, cb, ob * 128:(ob + 1) * 128],
                                        dd[:, dsl, tt * 512:(tt + 1) * 512], start=(cb == 0), stop=(cb == 1)),
                                       reads=[t_pw, t_dd[dsl]], writes=[pst[b]], inc=(cb == 1))
                                col = l * 8 + g * 2 + ob
                                op("dve", lambda e, b=b, osl=osl, tt=tt, col=col: e.tensor_scalar(
                                    out=ost[:, osl, tt * 512:(tt + 1) * 512], in0=ps[:, b, :],
                                    scalar1=pscale[:, col:col + 1], scalar2=None, op0=ALU.mult),
                                   reads=[pst[b], t_const], writes=[t_ost[osl]])
                            r0 = (g * 2 + ob) * 128
                            dma("sp", s_ypool.ap()[r0:r0 + 128, :], ost[:, osl, :], reads=[t_ost[osl]], writes=[T["ypool"]])

                    def g_mix():
                        for g4 in range(4):
                            dma("pool", pw[:, g4], pool_w[l, g4].rearrange("(cb p) n -> p cb n", p=128), writes=[t_pw])
                        pending = None
                        for blk in range(8):
                            load_blk(s_upool, T["upool"], 0, blk)
                        for blk in range(8):
                            g = blk // 2
                            w = WINS[g]
                            si = blk % 2
                            dsl = (g % 2) * 2 + si
                            halo_blk(blk)
                            cur, cur_t = ue[:, blk], t_ue[blk]
                            sh = 1
                            outs = ((a1, t_a1), (a2, t_a2))
                            oi = 0
                            while sh < w:
                                dst, dst_t = outs[oi]
                                op("dve", lambda e, cur=cur, dst=dst, sh=sh: e.tensor_tensor(
                                    out=dst[:, :, sh:272], in0=cur[:, :, sh:272], in1=cur[:, :, 0:272 - sh], op=ALU.add),
                                   reads=[cur_t], writes=[dst_t])
                                cur, cur_t = dst, dst_t
                                oi ^= 1
                                sh *= 2
                            op("dve", lambda e, cur=cur, blk=blk, dsl=dsl, w=w: e.scalar_tensor_tensor(
                                out=dd[:, dsl, :].rearrange("p (j w) -> p j w", j=4), in0=cur[:, :, 16:272], scalar=1.0 / w,
                                in1=ue[:, blk, :, 16:272], op0=ALU.mult, op1=ALU.subtract),
                               reads=[cur_t, t_ue[blk]], writes=[t_dd[dsl]])
                            op("dve", lambda e, cur=cur, g=g: e.tensor_tensor(out=dfix[:], in0=cur[:, :, 16:32],
                                                                              in1=invc[:, g], op=ALU.mult),
                               reads=[cur_t, t_const], writes=[t_dfix])
                            op("dve", lambda e, blk=blk, dsl=dsl: e.tensor_tensor(
                                out=dd[:, dsl, :].rearrange("p (j w) -> p j w", j=4)[:, :, 0:16], in0=dfix[:],
                                in1=ue[:, blk, :, 16:32], op=ALU.subtract),
                               reads=[t_dfix, t_ue[blk], t_dd[dsl]], writes=[t_dd[dsl]])
                            yield
                            if pending is not None and si == 0:
                                pool_mm(pending)
                                pending = None
                            if si == 1:
                                pending = g
                        for blk in range(8):
                            load_blk(s_z, T["z"], 1024, blk)
                            r0 = blk * 128
                            dma("sp", bcv[:, blk, :], s_bconv.ap()[r0:r0 + 128, :], reads=[T["bconv"]], writes=[t_bcv[blk]])
                        for blk in range(8):
                            si = blk % 2
                            r0 = blk * 128
                            halo_blk(blk)

                            def cw(k, blk=blk):
                                c = (l * 3 + k) * 8 + blk
                                return convw[:, c:c + 1]
                            op("dve", lambda e, blk=blk: e.tensor_scalar(out=acc[:], in0=ue[:, blk, :, 14:270], scalar1=cw(0),
                                                                         scalar2=None, op0=ALU.mult),
                               reads=[t_ue[blk], t_const], writes=[t_acc])
                            for k in (1, 2):
                                op("dve", lambda e, blk=blk, k=k: e.scalar_tensor_tensor(
                                    out=acc[:], in0=ue[:, blk, :, 14 + k:270 + k], scalar=cw(k), in1=acc[:],
                                    op0=ALU.mult, op1=ALU.add),
                                   reads=[t_ue[blk], t_const, t_acc], writes=[t_acc])
                            osl = si
                            op("dve", lambda e, blk=blk, osl=osl: e.tensor_tensor(
                                out=ost[:, osl, :].rearrange("p (j w) -> p j w", j=4), in0=acc[:],
                                in1=bcv[:, blk, :].rearrange("p (j w) -> p j w", j=4), op=ALU.mult),
                               reads=[t_acc, t_bcv[blk]], writes=[t_ost[osl]])
                            dma("sp", s_yconv.ap()[r0:r0 + 128, :], ost[:, osl, :], reads=[t_ost[osl]], writes=[T["yconv"]])
                            yield
                            if pending is not None:
                                pool_mm(pending)
                                pending = None

                    G1, G2 = g_gemm(), g_mix()
                    n1, n2 = 38, 16
                    d1 = d2 = 0
                    e1 = e2 = False
                    while not (e1 and e2):
                        lead = (d1 < 6)
                        if not e1 and (lead or e2 or d1 * n2 <= (d2 + 2) * n1 * 0.55):
                            try:
                                next(G1)
                                d1 += 1
                            except StopIteration:
                                e1 = True
                        elif not e2:
                            try:
                                next(G2)
                                d2 += 1
                            except StopIteration:
                                e2 = True
                        else:
                            e1 = e1 or e2
                            if not e1:
                                continue
                cx.barrier()

            cc = [_core_consts(r) for r in range(2)]
            near_any = [[False] * 16 for _ in range(8)]
            skip_all = [[True] * 16 for _ in range(8)]
            for r in range(2):
                qp, kp, sl_, _, _ = cc[r]
                sl3 = sl_[0].reshape(8, 16, 2)
                for qb_ in range(8):
                    qmax = qp[:, qb_].max()
                    for kb_ in range(16):
                        if sl3[qb_, kb_].any():
                            near_any[qb_][kb_] = True
                        if kp[0, kb_ * 128:(kb_ + 1) * 128].min() <= qmax:
                            skip_all[qb_][kb_] = False

            def attention(l):
                with contextlib.ExitStack() as es:
                    def A(name, shape, dtype):
                        return es.enter_context(cx.sbuf(name, shape, dtype))
                    tbb = A("tbb", [128, 2, 8, 128], BF16)
                    t_tb = Tok()
                    tbv = tb_in.rearrange("p (a h t) -> p a h t", a=3, h=8)
                    KT = A("KT", [128, 2, SEQ], BF16)
                    V = A("V", [128, 16, 2, 128], BF16)
                    kiT = A("kiT", [128, 2, SEQ], BF16)
                    kpos = A("kpos", [128, SEQ], mybir.dt.uint16)
                    t_K, t_V, t_VT, t_ki, t_kpos = Tok(), Tok(), Tok(), Tok(), Tok()
                    dma("sp", kpos[:], kpos_in[:, :], writes=[t_kpos])
                    op("dve", lambda e: e.memset(kiT[:], 0.0), writes=[t_ki])
                    xko = xk_out.ap()
                    with cx.sbuf("VT", [128, 2, SEQ], BF16) as VT, cx.sbuf("tbf", [128, 3, 8, 128], F32) as tbf:
                        t_tbf = Tok()
                        dma("sp", tbf[:], tbv, writes=[t_tbf])
                        for a in range(2):
                            op("dve", lambda e, a=a: e.tensor_tensor(out=tbb[:, a], in0=tbf[:, a], in1=tbf[:, 2],
                                                                     op=ALU.subtract),
                               reads=[t_tbf], writes=[t_tb])
                        for r in range(2):
                            for g in range(2):
                                for (dst, tk, row) in ((KT, t_K, 0), (VT, t_VT, 256)):
                                    rr = r * 576 + row + g * 128
                                    dma("sp", dst[:, g, :].rearrange("p (j r w) -> p j r w", j=4, r=2)[:, :, r, :],
                                        xko[rr:rr + 128, :].rearrange("p (j w) -> p j w", j=4),
                                        reads=[T["xk_out"]], writes=[tk])
                            for hh in range(2):
                                rr = r * 576 + 512
                                dma("sp", kiT[hh * 64:(hh + 1) * 64, 0, :].rearrange("p (j r w) -> p j r w", j=4, r=2)[:, :, r, :],
                                    xko[rr:rr + 64, :].rearrange("p (j w) -> p j w", j=4),
                                    reads=[T["xk_out"]], writes=[t_ki])
                        for kb4 in range(4):
                            for g in range(2):
                                pb = 0
                                for k in range(4):
                                    kb = kb4 * 4 + k
                                    op("pe", lambda e, kb=kb, g=g, pb=pb, k=k: e.transpose(
                                        psb[:, pb, k * 128:(k + 1) * 128], VT[:, g, kb * 128:(kb + 1) * 128], ident[:]),
                                       reads=[t_VT, t_const], writes=[psbt[pb]])
                                op("act", lambda e, kb4=kb4, g=g, pb=pb: e.activation(
                                    out=V[:, kb4 * 4:(kb4 + 1) * 4, g, :],
                                    in_=psb[:, pb, 0:512].rearrange("p (k d) -> p k d", k=4), func=AF.Copy),
                                   reads=[psbt[pb]], writes=[t_V])
                        cx.barrier()
                    qT = A("qT", [128, 2, 8, 128], BF16)
                    qiT = A("qiT", [128, 2, 8, 128], BF16)
                    score2 = [A("scoreA", [128, SEQ], F32), A("scoreB", [128, SEQ], F32)]
                    work = A("work", [128, SEQ], F32)
                    negb = A("negb", [128, SEQ], BF16)
                    mask = A("mask", [128, SEQ], BF16)
                    maskT = A("maskT", [128, 2, 16, 128], BF16)
                    rl = A("rl", [128, 4, 512], BF16)
                    pp = A("pp", [128, 2, 512], BF16)
                    od = A("od", [128, 2, 2, 512], F32)
                    selI = A("selI", [128, 8, 128], BF16)
                    mx = A("mx", [128, 8], F32)
                    thr = A("thr", [128, 1], F32)
                    aw = A("aw", [128, 16], F32)
                    sgn = A("sgn", [128, 16], F32)
                    Dg = A("Dg", [128, 16, 128], BF16)
                    rden = A("rden", [128, 512], F32)
                    yst = A("yst", [128, 2, 4, 128], BF16)
                    t_q = [Tok(), Tok()]
                    t_qi = [Tok(), Tok()]
                    t_score = [Tok(), Tok()]
                    t_work, t_negb, t_mask = Tok(), Tok(), Tok()
                    t_maskT = [Tok(), Tok()]
                    t_rl = [Tok() for _ in range(4)]
                    t_pp = [Tok(), Tok()]
                    t_od = [Tok(), Tok()]
                    t_selI = Tok()
                    t_mx, t_thr, t_rden, t_aw, t_D = Tok(), Tok(), Tok(), Tok(), Tok()
                    t_yst = [Tok(), Tok()]
                    B_ACC, B_D, B_O, B_DEN, B_Q = 0, (1, 2), 3, 4, (5, 6)

                    def load_qi(qb):
                        qs = qb % 2
                        dma("sp", qiT[:, qs], s_qi.ap().rearrange("(h p) t -> p h t", p=128)[:, :, qb * 128:(qb + 1) * 128],
                            reads=[T["qi"]], writes=[t_qi[qs]])

                    def load_qT(qb):
                        qs = qb % 2
                        dma("sp", qT[:, qs], s_q.ap().rearrange("(h p) t -> p h t", p=128)[:, :, qb * 128:(qb + 1) * 128],
                            reads=[T["q"]], writes=[t_q[qs]])

                    def nk_of(qb):
                        return 512 * (qb // 2 + 1)

                    def g_scores(qb):
                        qs = qb % 2
                        sc, t_sc = score2[qs], t_score[qs]
                        NK = nk_of(qb)
                        op("act", lambda e: e.activation(out=aw[:], in_=wi_sb[:, qb, :], func=AF.Abs),
                           reads=[t_wi], writes=[t_aw])
                        op("act", lambda e: e.activation(out=sgn[:], in_=wi_sb[:, qb, :], func=AF.Sign),
                           reads=[t_wi], writes=[t_aw])
                        for hd in range(16):
                            op("dve", lambda e, hd=hd: e.tensor_scalar(out=Dg[:, hd, :], in0=ident[:], scalar1=sgn[:, hd:hd + 1],
                                                                        scalar2=None, op0=ALU.mult),
                               reads=[t_aw, t_const], writes=[t_D])
                        op("dve", lambda e: e.tensor_scalar(out=negb[:, 0:NK], in0=kpos[:, 0:NK],
                                                            scalar1=qpos[:, qb:qb + 1], scalar2=NEG_MASK,
                                                            op0=ALU.is_gt, op1=ALU.mult),
                           reads=[t_kpos, t_const], writes=[t_negb])
                        yield
                        for kt in range(NK // 512):
                            ksl = slice(kt * 512, (kt + 1) * 512)
                            for hd in range(16):
                                hp, hh = hd // 2, hd % 2
                                b = B_D[hd % 2]
                                ri = hd % 4
                                op("pe", lambda e, b=b, hh=hh, hp=hp: e.matmul(
                                    ps[:, b, :], qiT[hh * 64:(hh + 1) * 64, qs, hp, :], kiT[hh * 64:(hh + 1) * 64, 0, ksl],
                                    start=True, stop=True),
                                   reads=[t_qi[qs], t_ki], writes=[pst[b]])
                                op("act", lambda e, b=b, ri=ri, hd=hd: e.activation(
                                    out=rl[:, ri, :], in_=ps[:, b, :], func=AF.Relu, scale=aw[:, hd:hd + 1]),
                                   reads=[pst[b], t_aw], writes=[t_rl[ri]])
                                op("pe", lambda e, ri=ri, hd=hd: e.matmul(
                                    ps[:, B_ACC, :], Dg[:, hd, :], rl[:, ri, :], start=(hd == 0), stop=False),
                                   reads=[t_D, t_rl[ri]], writes=[pst[B_ACC]], inc=False)
                                yield
                            op("pe", lambda e: e.matmul(ps[:, B_ACC, :], ident[:], negb[:, ksl], start=False, stop=True),
                               reads=[t_const, t_negb], writes=[pst[B_ACC]], inc=True)
                            op("act", lambda e: e.activation(out=sc[:, ksl], in_=ps[:, B_ACC, :], func=AF.Copy),
                               reads=[pst[B_ACC], t_sc], writes=[t_sc])

                    def g_topk(qb):
                        qs = qb % 2
                        sc, t_sc = score2[qs], t_score[qs]
                        NK = nk_of(qb)
                        nkb = NK // 128
                        for rd in range(32):
                            src = sc if rd == 0 else work
                            op("dve", lambda e, src=src: e.max(out=mx[:], in_=src[:, 0:NK]),
                               reads=[t_sc, t_work], writes=[t_mx])
                            if rd < 31:
                                op("dve", lambda e, src=src: e.match_replace(out=work[:, 0:NK], in_to_replace=mx[:],
                                                                              in_values=src[:, 0:NK], imm_value=NEG_REPL),
                                   reads=[t_mx, t_sc], writes=[t_work])
                            yield
                        yield "tail"
                        op("dve", lambda e: e.tensor_scalar(out=thr[:], in0=mx[:, 7:8], scalar1=-1.0e29, scalar2=None,
                                                            op0=ALU.max),
                           reads=[t_mx], writes=[t_thr])
                        op("dve", lambda e: e.tensor_scalar(out=mask[:, 0:NK], in0=sc[:, 0:NK], scalar1=thr[:, 0:1],
                                                            scalar2=None, op0=ALU.is_ge),
                           reads=[t_sc, t_thr], writes=[t_mask])
                        if debug:
                            dma("sp", s_dbgmask.ap()[qb * 128:(qb + 1) * 128, 0:NK], mask[:, 0:NK], reads=[t_mask], writes=[T["yout"]])
                        for k4 in range(nkb // 4):
                            pb = 0
                            for k in range(4):
                                kb = k4 * 4 + k
                                op("pe", lambda e, kb=kb, pb=pb, k=k: e.transpose(
                                    psb[:, pb, k * 128:(k + 1) * 128], mask[:, kb * 128:(kb + 1) * 128], ident[:]),
                                   reads=[t_mask, t_const], writes=[psbt[pb]])
                            op("act", lambda e, k4=k4, pb=pb: e.activation(
                                out=maskT[:, qs, k4 * 4:(k4 + 1) * 4, :],
                                in_=psb[:, pb, 0:512].rearrange("p (k d) -> p k d", k=4), func=AF.Identity,
                                scale=30000.0, bias=negc[:, 0:1]),
                               reads=[psbt[pb], t_const], writes=[t_maskT[qs]])

                    def kbs_of(qb):
                        return [kb for kb in range(nk_of(qb) // 128) if not skip_all[qb][kb]]

                    def g_pv(qb):
                        qs = qb % 2
                        kbs = kbs_of(qb)
                        nears = [kb for kb in kbs if near_any[qb][kb]]
                        assert len(nears) <= 4
                        for ni, kb in enumerate(nears):
                            for a in range(2):
                                op("dve", lambda e, ni=ni, kb=kb, a=a: e.tensor_scalar(
                                    out=selI[:, ni * 2 + a, :], in0=ident[:], scalar1=sel[:, qb, kb, a:a + 1],
                                    scalar2=None, op0=ALU.mult), reads=[t_const], writes=[t_selI])
                        for g in range(2):
                            bo, bd = B_O, B_DEN
                            for ii, kb in enumerate(kbs):
                                si = ii % 2
                                bq = B_Q[ii % 2]
                                pq = ps[:, bq, :].rearrange("p (h t) -> p h t", h=4)
                                op("pe", lambda e, kb=kb, g=g: e.matmul(
                                    pq, KT[:, g, kb * 128:(kb + 1) * 128],
                                    qT[:, qs, 4 * g:4 * g + 4, :], start=True, stop=False),
                                   reads=[t_K, t_q[qs]], writes=[pst[bq]], inc=False)
                                if kb in nears:
                                    ni = nears.index(kb)
                                    for a in range(2):
                                        op("pe", lambda e, ni=ni, a=a, g=g: e.matmul(
                                            pq, selI[:, ni * 2 + a, :], tbb[:, a, 4 * g:4 * g + 4, :], start=False, stop=False),
                                           reads=[t_selI, t_tb], writes=[pst[bq]], inc=False)
                                op("pe", lambda e, kb=kb: e.matmul(
                                    pq, ident[:], maskT[:, qs, kb:kb + 1, :].to_broadcast([128, 4, 128]),
                                    start=False, stop=True),
                                   reads=[t_const, t_maskT[qs]], writes=[pst[bq]], inc=True)
                                op("act", lambda e, si=si, bq=bq: e.activation(out=pp[:, si, :], in_=ps[:, bq, :], func=AF.Exp),
                                   reads=[pst[bq]], writes=[t_pp[si]])
                                first = (ii == 0)
                                last = (ii == len(kbs) - 1)
                                op("pe", lambda e, kb=kb, g=g, si=si, first=first, last=last: e.matmul(
                                    ps[:, bo, :], V[:, kb, g, :], pp[:, si, :], start=first, stop=last),
                                   reads=[t_V, t_pp[si]], writes=[pst[bo]], inc=last)
                                op("pe", lambda e, kb=kb, si=si, first=first, last=last: e.matmul(
                                    ps[:, bd, :], ones[:], pp[:, si, :], start=first, stop=last),
                                   reads=[t_const, t_pp[si]], writes=[pst[bd]], inc=True)
                                yield
                            op("act", lambda e, g=g: e.activation(out=od[:, g, 0, :], in_=ps[:, bo, :], func=AF.Copy),
                               reads=[pst[bo]], writes=[t_od[g]])
                            op("act", lambda e, g=g: e.activation(out=od[:, g, 1, :], in_=ps[:, bd, :], func=AF.Copy),
                               reads=[pst[bd]], writes=[t_od[g]])
                            yield
                        for g in range(2):
                            op("dve", lambda e, g=g: e.reciprocal(out=rden[:], in_=od[:, g, 1, :]),
                               reads=[t_od[g]], writes=[t_rden])
                            op("dve", lambda e, g=g: e.tensor_tensor(
                                out=yst[:, g].rearrange("p h t -> p (h t)"), in0=od[:, g, 0, :], in1=rden[:], op=ALU.mult),
                               reads=[t_od[g], t_rden], writes=[t_yst[g]])
                            dma("sp", s_yattn.ap().rearrange("(h p) t -> p h t", p=128)[:, 4 * g:4 * g + 4, qb * 128:(qb + 1) * 128],
                                yst[:, g], reads=[t_yst[g]], writes=[T["yattn"]])
                        yield

                    for k in range(8 + 2):
                        gens = []
                        if k < 8:
                            load_qi(k)
                            gens.append([g_scores(k), (1 + 16 * (nk_of(k) // 512)) * 0.55, 0, False])
                        tgen = None
                        if 0 <= k - 1 < 8:
                            load_qT(k - 1)
                            tgen = g_topk(k - 1)
                            gens.append([tgen, 32, 0, False])
                        if 0 <= k - 2 < 8:
                            gens.append([g_pv(k - 2), (2 * len(kbs_of(k - 2)) + 3) * 0.8, 0, False])
                        while True:
                            live = [x for x in gens if not x[3]]
                            if not live:
                                break
                            x = min(live, key=lambda x: x[2] / x[1])
                            try:
                                y = next(x[0])
                                x[2] += 1
                                if y == "tail":
                                    x[3] = True
                            except StopIteration:
                                x[3] = True
                        if tgen is not None:
                            for _ in tgen:
                                pass
                cx.barrier()

            def post_norm_residual(l, gi, m, t_m, tt, final):
                tsl = slice(tt * 512, (tt + 1) * 512)
                with cx.sbuf("psq", [128, 2, 512], BF16) as psq, \
                        cx.sbuf("prr", [128, 512], F32) as prr, \
                        cx.sbuf("pxb", [128, 2, 512], F32) as pxb, \
                        cx.sbuf("ptm", [128, 2, 512], F32) as ptm:
                    t_psq, t_prr = [Tok(), Tok()], Tok()
                    t_pxb = [Tok(), Tok()]
                    t_ptm = [Tok(), Tok()]
                    b = bank()
                    for kb in range(NB):
                        op("act", lambda e, kb=kb: e.activation(out=psq[:, kb % 2, :], in_=m[:, kb, :], func=AF.Square),
                           reads=[t_m], writes=[t_psq[kb % 2]])
                        op("pe", lambda e, kb=kb, b=b: e.matmul(ps[:, b, :], ones[:], psq[:, kb % 2, :],
                                                               start=(kb == 0), stop=(kb == NB - 1)),
                           reads=[t_psq[kb % 2], t_const], writes=[pst[b]], inc=True)
                    op("act", lambda e, b=b: e.activation(out=prr[:], in_=ps[:, b, :], func=AF.Sqrt, bias=EPS,
                                                          scale=1.0 / D), reads=[pst[b]], writes=[t_prr])
                    op("dve", lambda e: e.reciprocal(out=prr[:], in_=prr[:]), reads=[t_prr], writes=[t_prr])
                    xv = xT.ap().rearrange("(kb p) t -> kb p t", p=128)
                    yv = yout.rearrange("(kb p) t -> kb p t", p=128)
                    for kb in range(NB):
                        si = kb % 2
                        dma("sp", pxb[:, si, :], xv[kb, :, tsl], reads=[TX[kb]], writes=[t_pxb[si]])
                        op("dve", lambda e, kb=kb, si=si: e.scalar_tensor_tensor(
                            out=ptm[:, si, :], in0=m[:, kb, :], scalar=gcol(l, gi, kb), in1=prr[:],
                            op0=ALU.mult, op1=ALU.mult), reads=[t_m, t_prr, t_const], writes=[t_ptm[si]])
                        op("dve", lambda e, si=si: e.tensor_tensor(out=ptm[:, si, :], in0=ptm[:, si, :],
                                                                   in1=pxb[:, si, :], op=ALU.add),
                           reads=[t_pxb[si], t_ptm[si]], writes=[t_ptm[si]])
                        if final:
                            dma("sp", yv[kb, :, tsl], ptm[:, si, :], reads=[t_ptm[si]], writes=[T["yout"]])
                        else:
                            dma("sp", xv[kb, :, tsl], ptm[:, si, :], reads=[t_ptm[si]], writes=[TX[kb]])

            def merge_out(l):
              with cx.sbuf("S", [128, NB, NT], BF16) as S:
                t_S = Tok()
                with contextlib.ExitStack() as es:
                    def A(name, shape, dtype):
                        return es.enter_context(cx.sbuf(name, shape, dtype))
                    yb = [A("yb%d" % n, [128, 8, NT], BF16) for n in range(3)]
                    accm = A("accm", [128, 2, NT], F32)
                    sg = A("sg", [128, 2, 512], F32)
                    tmpm = A("tmpm", [128, 2, 512], F32)
                    t_yb = [Tok(), Tok(), Tok()]
                    t_accm = [[Tok(), Tok()] for _ in range(2)]
                    t_sg = [Tok(), Tok()]
                    t_tmp = [Tok(), Tok()]
                    for n, (src, nm) in enumerate(((s_ypool, "ypool"), (s_yconv, "yconv"), (s_yattn, "yattn"))):
                        dma("sp", yb[n][:], src.ap().rearrange("(kb p) t -> p kb t", p=128), reads=[T[nm]], writes=[t_yb[n]])
                    rr = [0]
                    for c in range(8):
                        for n in range(3):
                            def gdst(buf):
                                return buf[:, 0:16 * 256].rearrange("p (kb n) -> p kb n", kb=16)
                            gsrc = w_gate[l].rearrange("(kb p) n -> p kb n", p=128)[:, :, n * D + c * 256:n * D + (c + 1) * 256]

                            def bdst(buf):
                                return buf[:, 16 * 256:24 * 256].rearrange("p (kb n) -> p kb n", kb=8)
                            bsrc = w_branch[l, n].rearrange("(kb p) n -> p kb n", p=128)[:, :, c * 256:(c + 1) * 256]
                            gbuf, gtok = ws.get("gb%d_%d_%d" % (c, n, l), [(gdst, gsrc), (bdst, bsrc)])
                            gv = gbuf[:, 0:16 * 256].rearrange("p (kb n) -> p kb n", kb=16)
                            bv = gbuf[:, 16 * 256:24 * 256].rearrange("p (kb n) -> p kb n", kb=8)
                            btok = gtok
                            for db in range(2):
                                dblk = c * 2 + db
                                for tt in range(2):
                                    tsl = slice(tt * 512, (tt + 1) * 512)
                                    bg = gemm_block(gv, gtok, 16, lambda kb, tsl=tsl: hT[:, kb, tsl], [t_hT[tt]], db * 128)
                                    bu = gemm_block(bv, btok, 8, lambda kb, tsl=tsl, n=n: yb[n][:, kb, tsl], [t_yb[n]], db * 128)
                                    si = rr[0]
                                    rr[0] ^= 1
                                    bc = (l * 3 + n) * NB + dblk
                                    op("act", lambda e, bg=bg, si=si, bc=bc: e.activation(
                                        out=sg[:, si, :], in_=ps[:, bg, :], func=AF.Sigmoid, bias=bgate[:, bc:bc + 1]),
                                       reads=[pst[bg], t_const], writes=[t_sg[si]])
                                    if n == 0:
                                        op("dve", lambda e, bu=bu, si=si, db=db, tsl=tsl: e.tensor_tensor(
                                            out=accm[:, db, tsl], in0=ps[:, bu, :], in1=sg[:, si, :], op=ALU.mult),
                                           reads=[pst[bu], t_sg[si]], writes=[t_accm[db][tt]])
                                    else:
                                        op("dve", lambda e, bu=bu, si=si: e.tensor_tensor(
                                            out=tmpm[:, si, :], in0=ps[:, bu, :], in1=sg[:, si, :], op=ALU.mult),
                                           reads=[pst[bu], t_sg[si]], writes=[t_tmp[si]])
                                        if n == 1:
                                            op("dve", lambda e, si=si, db=db, tsl=tsl: e.tensor_tensor(
                                                out=accm[:, db, tsl], in0=accm[:, db, tsl], in1=tmpm[:, si, :], op=ALU.add),
                                               reads=[t_tmp[si], t_accm[db][tt]], writes=[t_accm[db][tt]])
                                        else:
                                            op("dve", lambda e, si=si, db=db, tsl=tsl, dblk=dblk: e.tensor_tensor(
                                                out=S[:, dblk, tsl], in0=accm[:, db, tsl], in1=tmpm[:, si, :], op=ALU.add),
                                               reads=[t_tmp[si], t_accm[db][tt]], writes=[t_S])
                    cx.barrier()
                with cx.sbuf("mm", [128, NB, 512], F32) as mm:
                    t_mm = Tok()
                    for tt in range(2):
                        tsl = slice(tt * 512, (tt + 1) * 512)
                        for c in range(4):
                            def odst(buf):
                                return buf[:, :].rearrange("p (kb n) -> p kb n", kb=16)
                            osrc = w_out[l].rearrange("(kb p) n -> p kb n", p=128)[:, :, c * 512:(c + 1) * 512]
                            obuf, otok = ws.get("o%d_%d_%d" % (c, tt, l), [(odst, osrc)])
                            ov = obuf[:, :].rearrange("p (kb n) -> p kb n", kb=16)
                            for db in range(4):
                                dblk = c * 4 + db
                                b = gemm_block(ov, otok, 16, lambda kb: S[:, kb, tsl], [t_S], db * 128)
                                op("act", lambda e, b=b, dblk=dblk: e.activation(
                                    out=mm[:, dblk, :], in_=ps[:, b, :], func=AF.Copy),
                                   reads=[pst[b]], writes=[t_mm])
                        post_norm_residual(l, 1, mm, t_mm, tt, False)
                cx.barrier()

            def ffn(l, final):
              with cx.sbuf("actT", [128, 64, 512], BF16) as actT, \
                      cx.sbuf("fr", [128, 2, 512], F32) as fr, \
                      cx.sbuf("fm", [128, NB, 512], F32) as fm:
                t_act = Tok()
                t_fr = [Tok(), Tok()]
                t_fm = Tok()
                for tt in range(2):
                    tsl = slice(tt * 512, (tt + 1) * 512)
                    if True:
                        rr = 0
                        for c in range(16):
                            def udst(buf):
                                return buf[:, :].rearrange("p (kb n) -> p kb n", kb=16)
                            usrc = w_up[l].rearrange("(kb p) n -> p kb n", p=128)[:, :, c * 512:(c + 1) * 512]
                            ubuf, utok = ws.get("u%d_%d_%d" % (c, tt, l), [(udst, usrc)])
                            uv = ubuf[:, :].rearrange("p (kb n) -> p kb n", kb=16)
                            for hb in range(4):
                                b = gemm_block(uv, utok, 16, lambda kb: hT[:, kb, tsl], [t_hT[tt]], hb * 128)
                                si = rr
                                rr ^= 1
                                op("act", lambda e, b=b, si=si: e.activation(out=fr[:, si, :], in_=ps[:, b, :], func=AF.Relu),
                                   reads=[pst[b]], writes=[t_fr[si]])
                                hidx = c * 4 + hb
                                op("dve", lambda e, si=si, hidx=hidx: e.tensor_tensor(
                                    out=actT[:, hidx, :], in0=fr[:, si, :], in1=fr[:, si, :], op=ALU.mult),
                                   reads=[t_fr[si]], writes=[t_act])
                        for c in range(4):
                            banks = []
                            for db in range(4):
                                b = bank()
                                banks.append(b)
                            for hq in range(4):
                                def ddst(buf):
                                    return buf[:, :].rearrange("p (kb n) -> p kb n", kb=16)
                                dsrc = w_down[l].rearrange("(kb p) n -> p kb n", p=128)[:, hq * 16:(hq + 1) * 16, c * 512:(c + 1) * 512]
                                dbuf, dtok = ws.get("d%d_%d_%d_%d" % (c, hq, tt, l), [(ddst, dsrc)])
                                dv = dbuf[:, :].rearrange("p (kb n) -> p kb n", kb=16)
                                for db in range(4):
                                    b = banks[db]
                                    for kb in range(16):
                                        first = (hq == 0 and kb == 0)
                                        last = (hq == 3 and kb == 15)
                                        op("pe", lambda e, b=b, kb=kb, db=db, hq=hq, first=first, last=last: e.matmul(
                                            ps[:, b, :], dv[:, kb, db * 128:(db + 1) * 128], actT[:, hq * 16 + kb, :],
                                            start=first, stop=last),
                                           reads=[dtok, t_act], writes=[pst[b]], inc=(kb == 15))
                            for db in range(4):
                                b = banks[db]
                                dblk = c * 4 + db
                                op("act", lambda e, b=b, dblk=dblk: e.activation(out=fm[:, dblk, :], in_=ps[:, b, :], func=AF.Copy),
                                   reads=[pst[b]], writes=[t_fm])
                        post_norm_residual(l, 3, fm, t_fm, tt, final)
                cx.barrier()

            def scope(name):
                if cx.plan:
                    return contextlib.nullcontext()
                return nc.named_scope(name)
            for l in range(L):
                with scope("norm1"):
                    norm_to_h(l, 0)
                with scope("in_proj"):
                    in_proj(l)
                with scope("attention"):
                    attention(l)
                with scope("merge_out"):
                    merge_out(l)
                with scope("norm2"):
                    norm_to_h(l, 2)
                with scope("ffn"):
                    ffn(l, final=(l == L - 1))
            cx.barrier(engines=("sp", "act", "dve", "pe", "pool"))

    ws = WStream(cx, 3, 16 * 512)
    cx.plan = True
    emit_all()
    cx.plan = False
    cx.setup()
    ws.reset()
    with cx.stack:
        emit_all()
    return nc


def _t5_bucket(n):
    n = np.maximum(n, 0)
    nf = np.maximum(n, 1).astype(np.float32)
    large = 16 + (np.log(nf / 16) / math.log(128 / 16) * 16).astype(np.int32)
    large = np.minimum(large, 31)
    return np.where(n < 16, n, large)


def _core_consts(rank):
    mine = CHUNKS[rank]
    qabs = np.concatenate([np.arange(c * 256, (c + 1) * 256) for c in mine])
    qpos = qabs.reshape(8, 128).T.astype(np.float32).copy()
    kabs = np.concatenate([np.arange(CHUNKS[r][j] * 256, (CHUNKS[r][j] + 1) * 256)
                           for j in range(4) for r in range(2)])
    kpos = np.broadcast_to(kabs.astype(np.uint16), (128, SEQ)).copy()
    sel = np.zeros((8, 16, 2), np.float32)
    for qb in range(8):
        aq = qabs[qb * 128] // 128
        for kb in range(16):
            ak = kabs[kb * 128] // 128
            if aq - ak == 0:
                sel[qb, kb, 0] = 1.0
            elif aq - ak == 1:
                sel[qb, kb, 1] = 1.0
    sel = np.broadcast_to(sel.reshape(1, -1), (128, 256)).copy()
    selh = np.zeros((4, 4, 16), np.float32)
    for j in range(4):
        prev = mine[j] - 1
        for ci, (r, dj) in enumerate(((0, -1), (1, 0), (1, -1), (0, 0))):
            jj = j + dj
            if 0 <= jj < 4 and prev >= 0 and CHUNKS[r][jj] == prev:
                selh[ci, j, :] = 1.0
    selh = np.broadcast_to(selh.reshape(1, -1), (128, 256)).copy()
    invc = np.zeros((4, 4, 16), np.float32)
    for g, w in enumerate(WINS):
        for j in range(4):
            t = mine[j] * 256 + np.arange(16)
            invc[g, j] = 1.0 / np.minimum(t + 1, w)
    invc = np.broadcast_to(invc.reshape(1, -1), (128, 256)).copy()
    return qpos, kpos, sel, selh, invc


def _bias_tables(rel_bias):
    s = np.arange(128)[:, None]
    t = np.arange(128)[None, :]
    out = np.zeros((128, 3, 8, 128), np.float32)
    for a, R in enumerate((0, 128)):
        bk = _t5_bucket(R + t - s)
        out[:, a] = np.transpose(rel_bias[bk], (0, 2, 1))
    out[:, 2] = rel_bias[31][None, :, None]
    return out.reshape(128, -1).copy()


def _per_partition(v, L):
    v = np.asarray(v, np.float32)
    lead = v.shape[:-1]
    nblk = v.shape[-1] // 128
    a = v.reshape(lead + (nblk, 128))
    a = np.moveaxis(a, -1, 0)
    return np.ascontiguousarray(a.reshape(128, -1))


_NC_CACHE = {}


def _get_nc(L):
    if L not in _NC_CACHE:
        _NC_CACHE[L] = build(L)
    return _NC_CACHE[L]


def _run(L, xT_cores, layer0, p):
    nc = _get_nc(L)
    sl = slice(layer0, layer0 + L)
    in_maps = []
    tb = _bias_tables(np.asarray(p["rel_bias"], np.float32))
    ident = np.eye(128, dtype=np.float32).astype(ml_dtypes.bfloat16)
    shared = {
        "w_in": np.ascontiguousarray(p["w_in"][sl]), "pool_w": np.ascontiguousarray(p["pool_w"][sl]),
        "w_branch": np.ascontiguousarray(p["w_branch"][sl]), "w_gate": np.ascontiguousarray(p["w_gate"][sl]),
        "w_out": np.ascontiguousarray(p["w_out"][sl]), "w_up": np.ascontiguousarray(p["w_up"][sl]),
        "w_down": np.ascontiguousarray(p["w_down"][sl]),
        "gains": _per_partition(p["norm_gains"][sl], L), "pscale": _per_partition(p["pool_scale"][sl], L),
        "convw": _per_partition(p["conv_w"][sl], L), "bgate": _per_partition(p["b_gate"][sl], L),
        "tb": tb, "ident": ident,
    }
    for c in range(8):
        qpos, kpos, sel, selh, invc = _core_consts(c % 2)
        m = dict(shared)
        m.update({"xT": xT_cores[c], "qpos": qpos, "kpos": kpos, "sel": sel, "selh": selh, "invc": invc})
        in_maps.append(m)
    res = run_bass_kernel_spmd(nc, in_maps, core_ids=list(range(8)))
    return [np.asarray(r["yT"], np.float32) for r in res.results]


FUSED_LAYERS = 4


def kernel(x, norm_gains, w_in, pool_w, pool_scale, conv_w, rel_bias, w_branch, w_gate, b_gate,
           w_out, w_up, w_down):
    p = dict(norm_gains=np.asarray(norm_gains, np.float32), w_in=np.asarray(w_in, np.float32),
             pool_w=np.asarray(pool_w, np.float32), pool_scale=np.asarray(pool_scale, np.float32),
             conv_w=np.asarray(conv_w, np.float32), rel_bias=np.asarray(rel_bias, np.float32),
             w_branch=np.asarray(w_branch, np.float32), w_gate=np.asarray(w_gate, np.float32),
             b_gate=np.asarray(b_gate, np.float32), w_out=np.asarray(w_out, np.float32),
             w_up=np.asarray(w_up, np.float32), w_down=np.asarray(w_down, np.float32))
    x = np.asarray(x, np.float32)
    depth = p["w_in"].shape[0]
    xT = []
    for c in range(8):
        b, r = c // 2, c % 2
        toks = np.concatenate([x[b, ch * 256:(ch + 1) * 256] for ch in CHUNKS[r]], axis=0)
        xT.append(np.ascontiguousarray(toks.T))
    l0 = 0
    while l0 < depth:
        n = min(FUSED_LAYERS, depth - l0)
        xT = _run(n, xT, l0, p)
        l0 += n
    out = np.zeros_like(x)
    for c in range(8):
        b, r = c // 2, c % 2
        toks = xT[c].T
        for j, ch in enumerate(CHUNKS[r]):
            out[b, ch * 256:(ch + 1) * 256] = toks[j * 256:(j + 1) * 256]
    return out
```

```python
import math
import contextlib
import numpy as np
import ml_dtypes
import concourse.bass as bass
import concourse.mybir as mybir
from concourse.bass_utils import run_bass_kernel_spmd

F32 = mybir.dt.float32
BF16 = mybir.dt.bfloat16
AF = mybir.ActivationFunctionType
ALU = mybir.AluOpType

D = 2048
NT = 1024
SEQ = 2048
NB = 16
HID = 8192
INW = 6736
CHUNKS = ((0, 3, 4, 7), (1, 2, 5, 6))
WINS = (2, 4, 8, 16)
EPS = 1e-6
NEG_MASK = -3.0e38
NEG_REPL = -1.0e30


class Dummy:
    def __getitem__(self, k):
        return self

    def __getattr__(self, k):
        return self

    def __call__(self, *a, **k):
        return self


class Tok:
    __slots__ = ("w", "r")

    def __init__(self):
        self.w = None
        self.r = []


class Eng:
    def __init__(self, name, handle, sem):
        self.name = name
        self.h = handle
        self.sem = sem
        self.count = 0
        self.waited = {}


class Ctx:
    def __init__(self, nc):
        self.nc = nc
        self.plan = True
        self.stack = contextlib.ExitStack()

    def setup(self):
        nc = self.nc
        st = self.stack
        self.eng = {}
        for name, h in (("pe", nc.tensor), ("act", nc.scalar), ("dve", nc.vector),
                        ("pool", nc.gpsimd), ("sp", nc.sync)):
            sem = st.enter_context(nc.semaphore("s_" + name))
            self.eng[name] = Eng(name, h, sem)
        self.dsems = [st.enter_context(nc.semaphore("d%d" % i)) for i in range(40)]
        self.dcount = [0] * len(self.dsems)
        self.dnext = 0
        self.ccsem = st.enter_context(nc.semaphore("cc"))
        self.cccount = 0

    @contextlib.contextmanager
    def sbuf(self, name, shape, dtype):
        if self.plan:
            yield Dummy()
        else:
            self.uid = getattr(self, "uid", 0) + 1
            with self.nc.sbuf_tensor("sb%d_%s" % (self.uid, name), shape, dtype) as t:
                yield t

    def _waits(self, E, reads, writes):
        need = {}

        def add(ev):
            if ev is None:
                return
            sem, val = ev
            k = id(sem)
            if k not in need or need[k][1] < val:
                need[k] = (sem, val)
        for t in reads:
            add(t.w)
        for t in writes:
            add(t.w)
            for ev in t.r:
                add(ev)
        for k, (sem, val) in need.items():
            if E.name == "pe" and sem is E.sem:
                continue
            if E.waited.get(k, 0) < val:
                E.h.wait_ge(sem, val)
                E.waited[k] = val

    def _record(self, ev, reads, writes):
        for t in reads:
            t.r.append(ev)
            if len(t.r) > 64:
                t.r = t.r[-64:] if False else t.r
        for t in writes:
            t.w = ev
            t.r = []

    def op(self, en, emit, reads=(), writes=(), inc=True):
        if self.plan:
            return
        E = self.eng[en]
        self._waits(E, reads, writes)
        ins = emit(E.h)
        if inc:
            E.count += 1
            ins.then_inc(E.sem, 1)
            ev = (E.sem, E.count)
        else:
            ev = (E.sem, E.count + 1)
        self._record(ev, reads, writes)

    def dma(self, qn, out, in_, reads=(), writes=()):
        if self.plan:
            return
        E = self.eng[qn]
        self._waits(E, reads, writes)
        i = self.dnext
        self.dnext = (self.dnext + 1) % len(self.dsems)
        sem = self.dsems[i]
        k = id(sem)
        if self.dcount[i] > 0 and E.waited.get(k, 0) < self.dcount[i]:
            E.h.wait_ge(sem, self.dcount[i])
            E.waited[k] = self.dcount[i]
        E.h.dma_start(out=out, in_=in_).then_inc(sem, 16)
        self.dcount[i] += 16
        ev = (sem, self.dcount[i])
        self._record(ev, reads, writes)

    def allgather(self, in_t, out_t, reads=(), writes=()):
        if self.plan:
            return
        E = self.eng["pool"]
        self._waits(E, reads, writes)
        E.h.collective_compute("AllGather", ALU.bypass,
                               replica_groups=[[0, 1], [2, 3], [4, 5], [6, 7]],
                               ins=[in_t.ap().opt()], outs=[out_t.ap().opt()]).then_inc(self.ccsem)
        self.cccount += 1
        ev = (self.ccsem, self.cccount)
        self._record(ev, reads, writes)

    def barrier(self, engines=("pe", "act", "dve", "sp", "pool")):
        if self.plan:
            return
        evs = []
        for n in ("pe", "act", "dve", "pool"):
            E = self.eng[n]
            if E.count > 0:
                evs.append((E.sem, E.count))
        for i, sem in enumerate(self.dsems):
            if self.dcount[i] > 0:
                evs.append((sem, self.dcount[i]))
        for n in engines:
            E = self.eng[n]
            for sem, val in evs:
                if sem is E.sem:
                    continue
                k = id(sem)
                if E.waited.get(k, 0) < val:
                    E.h.wait_ge(sem, val)
                    E.waited[k] = val


class WStream:
    def __init__(self, cx, nbuf, elems):
        self.cx = cx
        self.nbuf = nbuf
        self.elems = elems
        self.planlist = []
        self.bufs = None
        self.toks = [Tok() for _ in range(nbuf)]
        self.reset()

    def reset(self):
        self.next_get = 0
        self.next_issue = 0

    def get(self, tag, dmas):
        cx = self.cx
        if cx.plan:
            self.planlist.append((tag, dmas))
            return Dummy(), Tok()
        i = self.next_get
        assert self.planlist[i][0] == tag, (self.planlist[i][0], tag)
        lim = min(len(self.planlist), i + self.nbuf)
        while self.next_issue < lim:
            j = self.next_issue
            buf = self.bufs[j % self.nbuf]
            tok = self.toks[j % self.nbuf]
            for dst_fn, src in self.planlist[j][1]:
                cx.dma("pool", dst_fn(buf), src, writes=[tok])
            self.next_issue += 1
        self.next_get += 1
        return self.bufs[i % self.nbuf], self.toks[i % self.nbuf]


def build(L, debug=False):
    nc = bass.Bass("TRN2", target_bir_lowering=False)
    dt0 = nc.dram_tensor

    def dt(name, shape, dtype, kind=None):
        if kind is not None:
            return dt0(name, shape, dtype, kind=kind)
        if debug and name.startswith(("s_", "xTs")):
            return dt0(name, shape, dtype, kind="ExternalOutput")
        return dt0(name, shape, dtype)

    def ext(name, shape, dtype=F32):
        return dt(name, list(shape), dtype, kind="ExternalInput").ap()

    xin = ext("xT", [D, NT])
    w_in = ext("w_in", [L, D, INW])
    pool_w = ext("pool_w", [L, 4, 256, 256])
    w_branch = ext("w_branch", [L, 3, 1024, D])
    w_gate = ext("w_gate", [L, D, 3 * D])
    w_out = ext("w_out", [L, D, D])
    w_up = ext("w_up", [L, D, HID])
    w_down = ext("w_down", [L, HID, D])
    gains_in = ext("gains", [128, L * 4 * NB])
    pscale_in = ext("pscale", [128, L * 8])
    convw_in = ext("convw", [128, L * 3 * 8])
    bgate_in = ext("bgate", [128, L * 3 * NB])
    tb_in = ext("tb", [128, 3 * 8 * 128])
    qpos_in = ext("qpos", [128, 8])
    kpos_in = ext("kpos", [128, SEQ], mybir.dt.uint16)
    sel_in = ext("sel", [128, 8 * 16 * 2])
    selh_in = ext("selh", [128, 4 * 4 * 16])
    invc_in = ext("invc", [128, 4 * 4 * 16])
    ident_in = ext("ident", [128, 128], BF16)
    yout = dt("yT", [D, NT], F32, kind="ExternalOutput").ap()

    xT = dt("xTs", [D, NT], F32)
    s_upool = dt("s_upool", [1024, NT], BF16)
    s_z = dt("s_z", [1024, NT], BF16)
    s_bconv = dt("s_bconv", [1024, NT], BF16)
    s_q = dt("s_q", [1024, NT], BF16)
    s_qi = dt("s_qi", [1024, NT], BF16)
    s_ypool = dt("s_ypool", [1024, NT], BF16)
    s_yconv = dt("s_yconv", [1024, NT], BF16)
    s_yattn = dt("s_yattn", [1024, NT], BF16)
    xk_in = dt("xk_in", [576, NT], BF16)
    xk_out = dt("xk_out", [1152, NT], BF16)
    xh_in = dt("xh_in", [2048, 64], BF16)
    xh_out = dt("xh_out", [4096, 64], BF16)
    s_dbgmask = dt("s_dbgmask", [1024, SEQ], BF16) if debug else None
    s_dbgsc = dt("s_dbgsc", [1024, SEQ], F32) if debug else None
    s_dbgK = dt("s_dbgK", [128, 2 * SEQ], BF16) if debug else None
    s_dbgV = dt("s_dbgV", [128, 16 * 2 * 128], BF16) if debug else None
    s_dbgX = dt("s_dbgX", [1152, NT], BF16) if debug else None
    s_dbglg = dt("s_dbglg", [8 * 2 * 16 * 128, 512], F32) if debug else None
    s_dbgpp = dt("s_dbgpp", [8 * 2 * 16 * 128, 512], BF16) if debug else None
    s_dbgod = dt("s_dbgod", [8 * 2 * 2 * 128, 512], F32) if debug else None
    TX = [Tok() for _ in range(NB)]
    T = {n: Tok() for n in ( "upool", "z", "bconv", "q", "qi", "ypool", "yconv", "yattn",
                            "xk_in", "xk_out", "xh_in", "xh_out", "yout")}

    cx = Ctx(nc)

    def emit_all():
        op = cx.op
        dma = cx.dma
        with contextlib.ExitStack() as top:
            def SB(name, shape, dtype):
                return top.enter_context(cx.sbuf(name, shape, dtype))
            hT = SB("hT", [128, NB, NT], BF16)
            slabs = [SB("slab%d" % i, [128, 16 * 512], BF16) for i in range(3)]
            ws.bufs = slabs
            gains = SB("gains", [128, L * 4 * NB], F32)
            pscale = SB("pscale", [128, L * 8], F32)
            convw = SB("convw", [128, L * 3 * 8], F32)
            bgate = SB("bgate", [128, L * 3 * NB], F32)
            qpos = SB("qpos", [128, 8], F32)
            sel = SB("sel", [128, 8, 16, 2], F32)
            selh = SB("selh", [128, 4, 4, 16], F32)
            invc = SB("invc", [128, 4, 4, 16], F32)
            ident = SB("ident", [128, 128], BF16)
            ones = SB("ones", [128, 128], BF16)
            negc = SB("negc", [128, 1], F32)
            wi_sb = SB("wi_sb", [128, 8, 16], F32)
            pw = SB("pw", [128, 4, 2, 256], BF16)
            t_pw = Tok()
            if cx.plan:
                ps = Dummy()
                psb = Dummy()
            else:
                ps = top.enter_context(nc.psum_tensor("ps", [128, 7, 512], F32))
                psb = top.enter_context(nc.psum_tensor("psb", [128, 1, 1024], BF16))
            pst = [Tok() for _ in range(7)]
            psbt = [Tok() for _ in range(2)]
            t_hT = [Tok(), Tok()]
            t_const = Tok()
            t_wi = Tok()

            for dst, src in ((gains, gains_in), (pscale, pscale_in), (convw, convw_in),
                             (bgate, bgate_in), (qpos, qpos_in), (ident, ident_in)):
                dma("sp", dst[:], src[:, :], writes=[t_const])
            dma("sp", sel[:], sel_in.rearrange("p (a b c) -> p a b c", a=8, b=16), writes=[t_const])
            dma("sp", selh[:], selh_in.rearrange("p (a b c) -> p a b c", a=4, b=4), writes=[t_const])
            dma("sp", invc[:], invc_in.rearrange("p (a b c) -> p a b c", a=4, b=4), writes=[t_const])
            op("dve", lambda e: e.memset(ones[:], 1.0), writes=[t_const])
            op("dve", lambda e: e.memset(negc[:], -30000.0), writes=[t_const])
            dma("sp", xT.ap()[:, :], xin[:, :], writes=TX)

            bank_rr = [0]

            def bank():
                b = bank_rr[0]
                bank_rr[0] = (b + 1) % 7
                return b

            def gcol(l, i, kb):
                return gains[:, (l * 4 + i) * NB + kb:(l * 4 + i) * NB + kb + 1]

            def norm_to_h(l, gi, tts=(0, 1)):
                with cx.sbuf("nx", [128, 2, NB, 512], F32) as nx2, \
                        cx.sbuf("nsq", [128, 2, NB, 512], BF16) as nsq2, \
                        cx.sbuf("nr", [128, 2, 512], F32) as nr2:
                    t_nx2, t_sq2, t_r2 = [Tok(), Tok()], [Tok(), Tok()], [Tok(), Tok()]
                    for tt in tts:
                        tsl = slice(tt * 512, (tt + 1) * 512)
                        dma("sp", nx2[:, tt], xT.ap().rearrange("(kb p) t -> p kb t", p=128)[:, :, tsl],
                            reads=TX, writes=[t_nx2[tt]])
                    for tt in tts:
                        op("act", lambda e, tt=tt: e.activation(out=nsq2[:, tt], in_=nx2[:, tt], func=AF.Square),
                           reads=[t_nx2[tt]], writes=[t_sq2[tt]])
                    for tt in tts:
                        tsl = slice(tt * 512, (tt + 1) * 512)
                        nx, nsq, nr = nx2[:, tt], nsq2[:, tt], nr2[:, tt]
                        t_nx, t_sq, t_r = t_nx2[tt], t_sq2[tt], t_r2[tt]
                        b = bank()
                        for kb in range(NB):
                            op("pe", lambda e, kb=kb, b=b: e.matmul(ps[:, b, :], ones[:], nsq[:, kb, :],
                                                                   start=(kb == 0), stop=(kb == NB - 1)),
                               reads=[t_sq, t_const], writes=[pst[b]], inc=(kb == NB - 1))
                        op("act", lambda e, b=b: e.activation(out=nr, in_=ps[:, b, :], func=AF.Sqrt,
                                                              bias=EPS, scale=1.0 / D),
                           reads=[pst[b]], writes=[t_r])
                        op("dve", lambda e: e.reciprocal(out=nr, in_=nr), reads=[t_r], writes=[t_r])
                        for kb in range(NB):
                            op("dve", lambda e, kb=kb: e.scalar_tensor_tensor(
                                out=hT[:, kb, tsl], in0=nx[:, kb, :], scalar=gcol(l, gi, kb), in1=nr,
                                op0=ALU.mult, op1=ALU.mult),
                               reads=[t_nx, t_r, t_const], writes=[t_hT[tt]])
                cx.barrier()

            def gemm_block(wbuf_view, wtok, nkb, rhs_fn, rhs_toks, col0, ncols=128):
                b = bank()
                for kb in range(nkb):
                    op("pe", lambda e, kb=kb, b=b: e.matmul(ps[0:ncols, b, :], wbuf_view[:, kb, col0:col0 + ncols],
                                                           rhs_fn(kb), start=(kb == 0), stop=(kb == nkb - 1)),
                       reads=[wtok] + rhs_toks, writes=[pst[b]], inc=(kb == nkb - 1))
                return b

            def win_slab(l, c0, ncols):
                def dst(buf):
                    return buf[:, 0:16 * ncols].rearrange("p (kb n) -> p kb n", kb=16)
                src = w_in[l].rearrange("(kb p) n -> p kb n", p=128)[:, :, c0:c0 + ncols]
                return [(dst, src)]

            def in_proj(l):
                with contextlib.ExitStack() as es:
                    def A(name, shape, dtype):
                        return es.enter_context(cx.sbuf(name, shape, dtype))
                    stg = A("stg", [128, 4, NT], BF16)
                    ustg = A("ustg", [128, 4, NT], F32)
                    ue = A("ue", [128, 8, 4, 272], BF16)
                    tl = A("tl", [128, 8, 2, 4, 16], BF16)
                    hf = A("hf", [128, 4, 16], F32)
                    htmp = A("htmp", [128, 4, 16], F32)
                    a1 = A("a1", [128, 4, 272], F32)
                    a2 = A("a2", [128, 4, 272], F32)
                    dd = A("dd", [128, 4, NT], BF16)
                    dfix = A("dfix", [128, 4, 16], F32)
                    acc = A("acc", [128, 4, 256], F32)
                    bcv = A("bcv", [128, 8, NT], BF16)
                    ost = A("ost", [128, 4, NT], BF16)
                    t_stg = [Tok() for _ in range(4)]
                    t_u = [Tok() for _ in range(4)]
                    stg_rr = [0]
                    t_ue = [Tok() for _ in range(8)]
                    t_tl = [Tok() for _ in range(8)]
                    t_hf, t_a1, t_a2, t_acc, t_dfix = Tok(), Tok(), Tok(), Tok(), Tok()
                    t_dd = [Tok() for _ in range(4)]
                    t_bcv = [Tok() for _ in range(8)]
                    t_ost = [Tok() for _ in range(4)]

                    def hrhs(tt):
                        return lambda kb: hT[:, kb, tt * 512:(tt + 1) * 512]

                    def plain(tag, c0, nblk, dst_t, dst_tok, row0, scale=1.0, tails=None):
                        buf, wtok = ws.get(tag, win_slab(l, c0, nblk * 128))
                        wv = buf[:, 0:16 * nblk * 128].rearrange("p (kb n) -> p kb n", kb=16)
                        for cb in range(nblk):
                            si = stg_rr[0]
                            stg_rr[0] = (si + 1) % 4
                            for tt in range(2):
                                b = gemm_block(wv, wtok, 16, hrhs(tt), [t_hT[tt]], cb * 128)
                                op("act", lambda e, b=b, si=si, tt=tt: e.activation(
                                    out=stg[:, si, tt * 512:(tt + 1) * 512], in_=ps[:, b, :], func=AF.Copy,
                                    scale=scale), reads=[pst[b]], writes=[t_stg[si]])
                            r0 = row0 + cb * 128
                            dma("sp", dst_t.ap()[r0:r0 + 128, :], stg[:, si, :], reads=[t_stg[si]], writes=[dst_tok])
                            if tails is not None:
                                tr0 = tails + cb * 128
                                dma("sp", xh_in.ap()[tr0:tr0 + 128, :].rearrange("p (j w) -> p j w", j=4),
                                    stg[:, si, :].rearrange("p (j w) -> p j w", j=4)[:, :, 240:256],
                                    reads=[t_stg[si]], writes=[T["xh_in"]])
                            yield

                    def drain(g):
                        for _ in g:
                            pass

                    drain(plain("pu0_%d" % l, 0, 4, s_upool, T["upool"], 0, tails=0))
                    drain(plain("pu1_%d" % l, 512, 4, s_upool, T["upool"], 512, tails=512))
                    for half in range(2):
                        buf, wtok = ws.get("cu%d_%d" % (half, l), win_slab(l, 1024 + half * 512, 512))
                        wv = buf[:, :].rearrange("p (kb n) -> p kb n", kb=16)
                        for cb in range(4):
                            for tt in range(2):
                                b = gemm_block(wv, wtok, 16, hrhs(tt), [t_hT[tt]], cb * 128)
                                op("act", lambda e, b=b, cb=cb, tt=tt: e.activation(
                                    out=ustg[:, cb, tt * 512:(tt + 1) * 512], in_=ps[:, b, :], func=AF.Copy),
                                   reads=[pst[b]], writes=[t_u[cb]])
                        buf, wtok = ws.get("cc%d_%d" % (half, l), win_slab(l, 2048 + half * 512, 512))
                        wv = buf[:, :].rearrange("p (kb n) -> p kb n", kb=16)
                        for cb in range(4):
                            si = stg_rr[0]
                            stg_rr[0] = (si + 1) % 4
                            for tt in range(2):
                                b = gemm_block(wv, wtok, 16, hrhs(tt), [t_hT[tt]], cb * 128)
                                op("dve", lambda e, b=b, cb=cb, tt=tt, si=si: e.tensor_tensor(
                                    out=stg[:, si, tt * 512:(tt + 1) * 512], in0=ps[:, b, :],
                                    in1=ustg[:, cb, tt * 512:(tt + 1) * 512], op=ALU.mult),
                                   reads=[pst[b], t_u[cb]], writes=[t_stg[si]])
                            r0 = half * 512 + cb * 128
                            dma("sp", s_z.ap()[r0:r0 + 128, :], stg[:, si, :], reads=[t_stg[si]], writes=[T["z"]])
                            dma("sp", xh_in.ap()[1024 + r0:1024 + r0 + 128, :].rearrange("p (j w) -> p j w", j=4),
                                stg[:, si, :].rearrange("p (j w) -> p j w", j=4)[:, :, 240:256],
                                reads=[t_stg[si]], writes=[T["xh_in"]])
                    cx.allgather(xh_in, xh_out, reads=[T["xh_in"]], writes=[T["xh_out"]])

                    def g_gemm():
                        yield from plain("kv%d" % l, 5120, 4, xk_in, T["xk_in"], 0)
                        buf, wtok = ws.get("kiwi%d" % l, win_slab(l, 6656, 80))
                        wv = buf[:, 0:16 * 80].rearrange("p (kb n) -> p kb n", kb=16)
                        si = stg_rr[0]
                        stg_rr[0] = (si + 1) % 4
                        for tt in range(2):
                            b = gemm_block(wv, wtok, 16, hrhs(tt), [t_hT[tt]], 0, ncols=64)
                            op("act", lambda e, b=b, si=si, tt=tt: e.activation(
                                out=stg[0:64, si, tt * 512:(tt + 1) * 512], in_=ps[0:64, b, :], func=AF.Copy),
                               reads=[pst[b]], writes=[t_stg[si]])
                        dma("sp", xk_in.ap()[512:576, :], stg[0:64, si, :], reads=[t_stg[si]], writes=[T["xk_in"]])
                        b = bank()
                        for qb in range(8):
                            for kb in range(NB):
                                op("pe", lambda e, kb=kb, qb=qb, b=b: e.matmul(
                                    ps[:, b, qb * 16:(qb + 1) * 16], hT[:, kb, qb * 128:(qb + 1) * 128],
                                    wv[:, kb, 64:80], start=(kb == 0), stop=(kb == NB - 1)),
                                   reads=[wtok, t_hT[qb // 4]], writes=[pst[b]], inc=(kb == NB - 1))
                        op("act", lambda e, b=b: e.activation(out=wi_sb[:].rearrange("p a b -> p (a b)"),
                                                              in_=ps[:, b, 0:128], func=AF.Copy, scale=0.25),
                           reads=[pst[b]], writes=[t_wi])
                        cx.allgather(xk_in, xk_out, reads=[T["xk_in"]], writes=[T["xk_out"]])
                        yield
                        yield from plain("cb0_%d" % l, 3072, 4, s_bconv, T["bconv"], 0)
                        yield from plain("cb1_%d" % l, 3584, 4, s_bconv, T["bconv"], 512)
                        yield from plain("q0_%d" % l, 4096, 4, s_q, T["q"], 0, scale=128 ** -0.5)
                        yield from plain("q1_%d" % l, 4608, 4, s_q, T["q"], 512, scale=128 ** -0.5)
                        yield from plain("qi0_%d" % l, 5632, 4, s_qi, T["qi"], 0)
                        yield from plain("qi1_%d" % l, 6144, 4, s_qi, T["qi"], 512)

                    def load_blk(src_t, src_tok, trow0, blk):
                        r0 = blk * 128
                        dma("sp", ue[:, blk, :, 16:272], src_t.ap()[r0:r0 + 128, :].rearrange("p (j w) -> p j w", j=4),
                            reads=[src_tok], writes=[t_ue[blk]])
                        for r in range(2):
                            rr = r * 2048 + trow0 + r0
                            dma("sp", tl[:, blk, r], xh_out.ap()[rr:rr + 128, :].rearrange("p (j w) -> p j w", j=4),
                                reads=[T["xh_out"]], writes=[t_tl[blk]])

                    def halo_blk(blk):
                        si = blk
                        cands = ((0, -1), (1, 0), (1, -1), (0, 0))
                        op("dve", lambda e: e.memset(hf[:], 0.0), writes=[t_hf])
                        for ci, (r, dj) in enumerate(cands):
                            j0 = 1 if dj == -1 else 0
                            srcv = tl[:, si, r, j0 + dj:4 + dj, :]
                            selv = selh[:, ci, j0:4, :]
                            op("dve", lambda e, srcv=srcv, selv=selv, j0=j0: e.tensor_tensor(
                                out=htmp[:, j0:4, :], in0=srcv, in1=selv, op=ALU.mult),
                               reads=[t_tl[si], t_const, t_hf], writes=[t_hf])
                            op("dve", lambda e, j0=j0: e.tensor_tensor(
                                out=hf[:, j0:4, :], in0=hf[:, j0:4, :], in1=htmp[:, j0:4, :], op=ALU.add),
                               reads=[t_hf], writes=[t_hf])
                        op("dve", lambda e, si=si: e.tensor_copy(out=ue[:, si, :, 0:16], in_=hf[:]),
                           reads=[t_hf, t_ue[si]], writes=[t_ue[si]])

                    def pool_mm(g):
                        for ob in range(2):
                            osl = (g % 2) * 2 + ob
                            for tt in range(2):
                                b = bank()
                                for cb in range(2):
                                    dsl = (g % 2) * 2 + cb
                                    op("pe", lambda e, b=b, cb=cb, ob=ob, tt=tt, dsl=dsl: e.matmul(
                                        ps[:, b, :], pw[:, g, cb, ob * 128:(ob + 1) * 128],
                                        dd[:, dsl, tt * 512:(tt + 1) * 512], start=(cb == 0), stop=(cb == 1)),
                                       reads=[t_pw, t_dd[dsl]], writes=[pst[b]], inc=(cb == 1))
                                col = l * 8 + g * 2 + ob
                                op("dve", lambda e, b=b, osl=osl, tt=tt, col=col: e.tensor_scalar(
                                    out=ost[:, osl, tt * 512:(tt + 1) * 512], in0=ps[:, b, :],
                                    scalar1=pscale[:, col:col + 1], scalar2=None, op0=ALU.mult),
                                   reads=[pst[b], t_const], writes=[t_ost[osl]])
                            r0 = (g * 2 + ob) * 128
                            dma("sp", s_ypool.ap()[r0:r0 + 128, :], ost[:, osl, :], reads=[t_ost[osl]], writes=[T["ypool"]])

                    def g_mix():
                        for g4 in range(4):
                            dma("pool", pw[:, g4], pool_w[l, g4].rearrange("(cb p) n -> p cb n", p=128), writes=[t_pw])
                        pending = None
                        for blk in range(8):
                            load_blk(s_upool, T["upool"], 0, blk)
                        for blk in range(8):
                            g = blk // 2
                            w = WINS[g]
                            si = blk % 2
                            dsl = (g % 2) * 2 + si
                            halo_blk(blk)
                            cur, cur_t = ue[:, blk], t_ue[blk]
                            sh = 1
                            outs = ((a1, t_a1), (a2, t_a2))
                            oi = 0
                            while sh < w:
                                dst, dst_t = outs[oi]
                                op("dve", lambda e, cur=cur, dst=dst, sh=sh: e.tensor_tensor(
                                    out=dst[:, :, sh:272], in0=cur[:, :, sh:272], in1=cur[:, :, 0:272 - sh], op=ALU.add),
                                   reads=[cur_t], writes=[dst_t])
                                cur, cur_t = dst, dst_t
                                oi ^= 1
                                sh *= 2
                            op("dve", lambda e, cur=cur, blk=blk, dsl=dsl, w=w: e.scalar_tensor_tensor(
                                out=dd[:, dsl, :].rearrange("p (j w) -> p j w", j=4), in0=cur[:, :, 16:272], scalar=1.0 / w,
                                in1=ue[:, blk, :, 16:272], op0=ALU.mult, op1=ALU.subtract),
                               reads=[cur_t, t_ue[blk]], writes=[t_dd[dsl]])
                            op("dve", lambda e, cur=cur, g=g: e.tensor_tensor(out=dfix[:], in0=cur[:, :, 16:32],
                                                                              in1=invc[:, g], op=ALU.mult),
                               reads=[cur_t, t_const], writes=[t_dfix])
                            op("dve", lambda e, blk=blk, dsl=dsl: e.tensor_tensor(
                                out=dd[:, dsl, :].rearrange("p (j w) -> p j w", j=4)[:, :, 0:16], in0=dfix[:],
                                in1=ue[:, blk, :, 16:32], op=ALU.subtract),
                               reads=[t_dfix, t_ue[blk], t_dd[dsl]], writes=[t_dd[dsl]])
                            yield
                            if pending is not None and si == 0:
                                pool_mm(pending)
                                pending = None
                            if si == 1:
                                pending = g
                        for blk in range(8):
                            load_blk(s_z, T["z"], 1024, blk)
                            r0 = blk * 128
                            dma("sp", bcv[:, blk, :], s_bconv.ap()[r0:r0 + 128, :], reads=[T["bconv"]], writes=[t_bcv[blk]])
                        for blk in range(8):
                            si = blk % 2
                            r0 = blk * 128
                            halo_blk(blk)

                            def cw(k, blk=blk):
                                c = (l * 3 + k) * 8 + blk
                                return convw[:, c:c + 1]
                            op("dve", lambda e, blk=blk: e.tensor_scalar(out=acc[:], in0=ue[:, blk, :, 14:270], scalar1=cw(0),
                                                                         scalar2=None, op0=ALU.mult),
                               reads=[t_ue[blk], t_const], writes=[t_acc])
                            for k in (1, 2):
                                op("dve", lambda e, blk=blk, k=k: e.scalar_tensor_tensor(
                                    out=acc[:], in0=ue[:, blk, :, 14 + k:270 + k], scalar=cw(k), in1=acc[:],
                                    op0=ALU.mult, op1=ALU.add),
                                   reads=[t_ue[blk], t_const, t_acc], writes=[t_acc])
                            osl = si
                            op("dve", lambda e, blk=blk, osl=osl: e.tensor_tensor(
                                out=ost[:, osl, :].rearrange("p (j w) -> p j w", j=4), in0=acc[:],
                                in1=bcv[:, blk, :].rearrange("p (j w) -> p j w", j=4), op=ALU.mult),
                               reads=[t_acc, t_bcv[blk]], writes=[t_ost[osl]])
                            dma("sp", s_yconv.ap()[r0:r0 + 128, :], ost[:, osl, :], reads=[t_ost[osl]], writes=[T["yconv"]])
                            yield
                            if pending is not None:
                                pool_mm(pending)
                                pending = None

                    G1, G2 = g_gemm(), g_mix()
                    n1, n2 = 38, 16
                    d1 = d2 = 0
                    e1 = e2 = False
                    while not (e1 and e2):
                        lead = (d1 < 6)
                        if not e1 and (lead or e2 or d1 * n2 <= (d2 + 2) * n1 * 0.55):
                            try:
                                next(G1)
                                d1 += 1
                            except StopIteration:
                                e1 = True
                        elif not e2:
                            try:
                                next(G2)
                                d2 += 1
                            except StopIteration:
                                e2 = True
                        else:
                            e1 = e1 or e2
                            if not e1:
                                continue
                cx.barrier()

            cc = [_core_consts(r) for r in range(2)]
            near_any = [[False] * 16 for _ in range(8)]
            skip_all = [[True] * 16 for _ in range(8)]
            for r in range(2):
                qp, kp, sl_, _, _ = cc[r]
                sl3 = sl_[0].reshape(8, 16, 2)
                for qb_ in range(8):
                    qmax = qp[:, qb_].max()
                    for kb_ in range(16):
                        if sl3[qb_, kb_].any():
                            near_any[qb_][kb_] = True
                        if kp[0, kb_ * 128:(kb_ + 1) * 128].min() <= qmax:
                            skip_all[qb_][kb_] = False

            def attention(l):
                with contextlib.ExitStack() as es:
                    def A(name, shape, dtype):
                        return es.enter_context(cx.sbuf(name, shape, dtype))
                    tbb = A("tbb", [128, 2, 8, 128], BF16)
                    t_tb = Tok()
                    tbv = tb_in.rearrange("p (a h t) -> p a h t", a=3, h=8)
                    KT = A("KT", [128, 2, SEQ], BF16)
                    V = A("V", [128, 16, 2, 128], BF16)
                    kiT = A("kiT", [128, 2, SEQ], BF16)
                    kpos = A("kpos", [128, SEQ], mybir.dt.uint16)
                    t_K, t_V, t_VT, t_ki, t_kpos = Tok(), Tok(), Tok(), Tok(), Tok()
                    dma("sp", kpos[:], kpos_in[:, :], writes=[t_kpos])
                    op("dve", lambda e: e.memset(kiT[:], 0.0), writes=[t_ki])
                    xko = xk_out.ap()
                    with cx.sbuf("VT", [128, 2, SEQ], BF16) as VT, cx.sbuf("tbf", [128, 3, 8, 128], F32) as tbf:
                        t_tbf = Tok()
                        dma("sp", tbf[:], tbv, writes=[t_tbf])
                        for a in range(2):
                            op("dve", lambda e, a=a: e.tensor_tensor(out=tbb[:, a], in0=tbf[:, a], in1=tbf[:, 2],
                                                                     op=ALU.subtract),
                               reads=[t_tbf], writes=[t_tb])
                        for r in range(2):
                            for g in range(2):
                                for (dst, tk, row) in ((KT, t_K, 0), (VT, t_VT, 256)):
                                    rr = r * 576 + row + g * 128
                                    dma("sp", dst[:, g, :].rearrange("p (j r w) -> p j r w", j=4, r=2)[:, :, r, :],
                                        xko[rr:rr + 128, :].rearrange("p (j w) -> p j w", j=4),
                                        reads=[T["xk_out"]], writes=[tk])
                            for hh in range(2):
                                rr = r * 576 + 512
                                dma("sp", kiT[hh * 64:(hh + 1) * 64, 0, :].rearrange("p (j r w) -> p j r w", j=4, r=2)[:, :, r, :],
                                    xko[rr:rr + 64, :].rearrange("p (j w) -> p j w", j=4),
                                    reads=[T["xk_out"]], writes=[t_ki])
                        for kb4 in range(4):
                            for g in range(2):
                                pb = 0
                                for k in range(4):
                                    kb = kb4 * 4 + k
                                    op("pe", lambda e, kb=kb, g=g, pb=pb, k=k: e.transpose(
                                        psb[:, pb, k * 128:(k + 1) * 128], VT[:, g, kb * 128:(kb + 1) * 128], ident[:]),
                                       reads=[t_VT, t_const], writes=[psbt[pb]])
                                op("act", lambda e, kb4=kb4, g=g, pb=pb: e.activation(
                                    out=V[:, kb4 * 4:(kb4 + 1) * 4, g, :],
                                    in_=psb[:, pb, 0:512].rearrange("p (k d) -> p k d", k=4), func=AF.Copy),
                                   reads=[psbt[pb]], writes=[t_V])
                        cx.barrier()
                    qT = A("qT", [128, 2, 8, 128], BF16)
                    qiT = A("qiT", [128, 2, 8, 128], BF16)
                    score2 = [A("scoreA", [128, SEQ], F32), A("scoreB", [128, SEQ], F32)]
                    work = A("work", [128, SEQ], F32)
                    negb = A("negb", [128, SEQ], BF16)
                    mask = A("mask", [128, SEQ], BF16)
                    maskT = A("maskT", [128, 2, 16, 128], BF16)
                    rl = A("rl", [128, 4, 512], BF16)
                    pp = A("pp", [128, 2, 512], BF16)
                    od = A("od", [128, 2, 2, 512], F32)
                    selI = A("selI", [128, 8, 128], BF16)
                    mx = A("mx", [128, 8], F32)
                    thr = A("thr", [128, 1], F32)
                    aw = A("aw", [128, 16], F32)
                    sgn = A("sgn", [128, 16], F32)
                    Dg = A("Dg", [128, 16, 128], BF16)
                    rden = A("rden", [128, 512], F32)
                    yst = A("yst", [128, 2, 4, 128], BF16)
                    t_q = [Tok(), Tok()]
                    t_qi = [Tok(), Tok()]
                    t_score = [Tok(), Tok()]
                    t_work, t_negb, t_mask = Tok(), Tok(), Tok()
                    t_maskT = [Tok(), Tok()]
                    t_rl = [Tok() for _ in range(4)]
                    t_pp = [Tok(), Tok()]
                    t_od = [Tok(), Tok()]
                    t_selI = Tok()
                    t_mx, t_thr, t_rden, t_aw, t_D = Tok(), Tok(), Tok(), Tok(), Tok()
                    t_yst = [Tok(), Tok()]
                    B_ACC, B_D, B_O, B_DEN, B_Q = 0, (1, 2), 3, 4, (5, 6)

                    def load_qi(qb):
                        qs = qb % 2
                        dma("sp", qiT[:, qs], s_qi.ap().rearrange("(h p) t -> p h t", p=128)[:, :, qb * 128:(qb + 1) * 128],
                            reads=[T["qi"]], writes=[t_qi[qs]])

                    def load_qT(qb):
                        qs = qb % 2
                        dma("sp", qT[:, qs], s_q.ap().rearrange("(h p) t -> p h t", p=128)[:, :, qb * 128:(qb + 1) * 128],
                            reads=[T["q"]], writes=[t_q[qs]])

                    def nk_of(qb):
                        return 512 * (qb // 2 + 1)

                    def g_scores(qb):
                        qs = qb % 2
                        sc, t_sc = score2[qs], t_score[qs]
                        NK = nk_of(qb)
                        op("act", lambda e: e.activation(out=aw[:], in_=wi_sb[:, qb, :], func=AF.Abs),
                           reads=[t_wi], writes=[t_aw])
                        op("act", lambda e: e.activation(out=sgn[:], in_=wi_sb[:, qb, :], func=AF.Sign),
                           reads=[t_wi], writes=[t_aw])
                        for hd in range(16):
                            op("dve", lambda e, hd=hd: e.tensor_scalar(out=Dg[:, hd, :], in0=ident[:], scalar1=sgn[:, hd:hd + 1],
                                                                        scalar2=None, op0=ALU.mult),
                               reads=[t_aw, t_const], writes=[t_D])
                        op("dve", lambda e: e.tensor_scalar(out=negb[:, 0:NK], in0=kpos[:, 0:NK],
                                                            scalar1=qpos[:, qb:qb + 1], scalar2=NEG_MASK,
                                                            op0=ALU.is_gt, op1=ALU.mult),
                           reads=[t_kpos, t_const], writes=[t_negb])
                        yield
                        for kt in range(NK // 512):
                            ksl = slice(kt * 512, (kt + 1) * 512)
                            for hd in range(16):
                                hp, hh = hd // 2, hd % 2
                                b = B_D[hd % 2]
                                ri = hd % 4
                                op("pe", lambda e, b=b, hh=hh, hp=hp: e.matmul(
                                    ps[:, b, :], qiT[hh * 64:(hh + 1) * 64, qs, hp, :], kiT[hh * 64:(hh + 1) * 64, 0, ksl],
                                    start=True, stop=True),
                                   reads=[t_qi[qs], t_ki], writes=[pst[b]])
                                op("act", lambda e, b=b, ri=ri, hd=hd: e.activation(
                                    out=rl[:, ri, :], in_=ps[:, b, :], func=AF.Relu, scale=aw[:, hd:hd + 1]),
                                   reads=[pst[b], t_aw], writes=[t_rl[ri]])
                                op("pe", lambda e, ri=ri, hd=hd: e.matmul(
                                    ps[:, B_ACC, :], Dg[:, hd, :], rl[:, ri, :], start=(hd == 0), stop=False),
                                   reads=[t_D, t_rl[ri]], writes=[pst[B_ACC]], inc=False)
                                yield
                            op("pe", lambda e: e.matmul(ps[:, B_ACC, :], ident[:], negb[:, ksl], start=False, stop=True),
                               reads=[t_const, t_negb], writes=[pst[B_ACC]], inc=True)
                            op("act", lambda e: e.activation(out=sc[:, ksl], in_=ps[:, B_ACC, :], func=AF.Copy),
                               reads=[pst[B_ACC], t_sc], writes=[t_sc])

                    def g_topk(qb):
                        qs = qb % 2
                        sc, t_sc = score2[qs], t_score[qs]
                        NK = nk_of(qb)
                        nkb = NK // 128
                        for rd in range(32):
                            src = sc if rd == 0 else work
                            op("dve", lambda e, src=src: e.max(out=mx[:], in_=src[:, 0:NK]),
                               reads=[t_sc, t_work], writes=[t_mx])
                            if rd < 31:
                                op("dve", lambda e, src=src: e.match_replace(out=work[:, 0:NK], in_to_replace=mx[:],
                                                                              in_values=src[:, 0:NK], imm_value=NEG_REPL),
                                   reads=[t_mx, t_sc], writes=[t_work])
                            yield
                        yield "tail"
                        op("dve", lambda e: e.tensor_scalar(out=thr[:], in0=mx[:, 7:8], scalar1=-1.0e29, scalar2=None,
                                                            op0=ALU.max),
                           reads=[t_mx], writes=[t_thr])
                        op("dve", lambda e: e.tensor_scalar(out=mask[:, 0:NK], in0=sc[:, 0:NK], scalar1=thr[:, 0:1],
                                                            scalar2=None, op0=ALU.is_ge),
                           reads=[t_sc, t_thr], writes=[t_mask])
                        if debug:
                            dma("sp", s_dbgmask.ap()[qb * 128:(qb + 1) * 128, 0:NK], mask[:, 0:NK], reads=[t_mask], writes=[T["yout"]])
                        for k4 in range(nkb // 4):
                            pb = 0
                            for k in range(4):
                                kb = k4 * 4 + k
                                op("pe", lambda e, kb=kb, pb=pb, k=k: e.transpose(
                                    psb[:, pb, k * 128:(k + 1) * 128], mask[:, kb * 128:(kb + 1) * 128], ident[:]),
                                   reads=[t_mask, t_const], writes=[psbt[pb]])
                            op("act", lambda e, k4=k4, pb=pb: e.activation(
                                out=maskT[:, qs, k4 * 4:(k4 + 1) * 4, :],
                                in_=psb[:, pb, 0:512].rearrange("p (k d) -> p k d", k=4), func=AF.Identity,
                                scale=30000.0, bias=negc[:, 0:1]),
                               reads=[psbt[pb], t_const], writes=[t_maskT[qs]])

                    def kbs_of(qb):
                        return [kb for kb in range(nk_of(qb) // 128) if not skip_all[qb][kb]]

                    def g_pv(qb):
                        qs = qb % 2
                        kbs = kbs_of(qb)
                        nears = [kb for kb in kbs if near_any[qb][kb]]
                        assert len(nears) <= 4
                        for ni, kb in enumerate(nears):
                            for a in range(2):
                                op("dve", lambda e, ni=ni, kb=kb, a=a: e.tensor_scalar(
                                    out=selI[:, ni * 2 + a, :], in0=ident[:], scalar1=sel[:, qb, kb, a:a + 1],
                                    scalar2=None, op0=ALU.mult), reads=[t_const], writes=[t_selI])
                        for g in range(2):
                            bo, bd = B_O, B_DEN
                            for ii, kb in enumerate(kbs):
                                si = ii % 2
                                bq = B_Q[ii % 2]
                                pq = ps[:, bq, :].rearrange("p (h t) -> p h t", h=4)
                                op("pe", lambda e, kb=kb, g=g: e.matmul(
                                    pq, KT[:, g, kb * 128:(kb + 1) * 128],
                                    qT[:, qs, 4 * g:4 * g + 4, :], start=True, stop=False),
                                   reads=[t_K, t_q[qs]], writes=[pst[bq]], inc=False)
                                if kb in nears:
                                    ni = nears.index(kb)
                                    for a in range(2):
                                        op("pe", lambda e, ni=ni, a=a, g=g: e.matmul(
                                            pq, selI[:, ni * 2 + a, :], tbb[:, a, 4 * g:4 * g + 4, :], start=False, stop=False),
                                           reads=[t_selI, t_tb], writes=[pst[bq]], inc=False)
                                op("pe", lambda e, kb=kb: e.matmul(
                                    pq, ident[:], maskT[:, qs, kb:kb + 1, :].to_broadcast([128, 4, 128]),
                                    start=False, stop=True),
                                   reads=[t_const, t_maskT[qs]], writes=[pst[bq]], inc=True)
                                op("act", lambda e, si=si, bq=bq: e.activation(out=pp[:, si, :], in_=ps[:, bq, :], func=AF.Exp),
                                   reads=[pst[bq]], writes=[t_pp[si]])
                                first = (ii == 0)
                                last = (ii == len(kbs) - 1)
                                op("pe", lambda e, kb=kb, g=g, si=si, first=first, last=last: e.matmul(
                                    ps[:, bo, :], V[:, kb, g, :], pp[:, si, :], start=first, stop=last),
                                   reads=[t_V, t_pp[si]], writes=[pst[bo]], inc=last)
                                op("pe", lambda e, kb=kb, si=si, first=first, last=last: e.matmul(
                                    ps[:, bd, :], ones[:], pp[:, si, :], start=first, stop=last),
                                   reads=[t_const, t_pp[si]], writes=[pst[bd]], inc=True)
                                yield
                            op("act", lambda e, g=g: e.activation(out=od[:, g, 0, :], in_=ps[:, bo, :], func=AF.Copy),
                               reads=[pst[bo]], writes=[t_od[g]])
                            op("act", lambda e, g=g: e.activation(out=od[:, g, 1, :], in_=ps[:, bd, :], func=AF.Copy),
                               reads=[pst[bd]], writes=[t_od[g]])
                            yield
                        for g in range(2):
                            op("dve", lambda e, g=g: e.reciprocal(out=rden[:], in_=od[:, g, 1, :]),
                               reads=[t_od[g]], writes=[t_rden])
                            op("dve", lambda e, g=g: e.tensor_tensor(
                                out=yst[:, g].rearrange("p h t -> p (h t)"), in0=od[:, g, 0, :], in1=rden[:], op=ALU.mult),
                               reads=[t_od[g], t_rden], writes=[t_yst[g]])
                            dma("sp", s_yattn.ap().rearrange("(h p) t -> p h t", p=128)[:, 4 * g:4 * g + 4, qb * 128:(qb + 1) * 128],
                                yst[:, g], reads=[t_yst[g]], writes=[T["yattn"]])
                        yield

                    for k in range(8 + 2):
                        gens = []
                        if k < 8:
                            load_qi(k)
                            gens.append([g_scores(k), (1 + 16 * (nk_of(k) // 512)) * 0.55, 0, False])
                        tgen = None
                        if 0 <= k - 1 < 8:
                            load_qT(k - 1)
                            tgen = g_topk(k - 1)
                            gens.append([tgen, 32, 0, False])
                        if 0 <= k - 2 < 8:
                            gens.append([g_pv(k - 2), (2 * len(kbs_of(k - 2)) + 3) * 0.8, 0, False])
                        while True:
                            live = [x for x in gens if not x[3]]
                            if not live:
                                break
                            x = min(live, key=lambda x: x[2] / x[1])
                            try:
                                y = next(x[0])
                                x[2] += 1
                                if y == "tail":
                                    x[3] = True
                            except StopIteration:
                                x[3] = True
                        if tgen is not None:
                            for _ in tgen:
                                pass
                cx.barrier()

            def post_norm_residual(l, gi, m, t_m, tt, final):
                tsl = slice(tt * 512, (tt + 1) * 512)
                with cx.sbuf("psq", [128, 2, 512], BF16) as psq, \
                        cx.sbuf("prr", [128, 512], F32) as prr, \
                        cx.sbuf("pxb", [128, 4, 512], F32) as pxb:
                    t_psq, t_prr = [Tok(), Tok()], Tok()
                    t_pxb = [Tok() for _ in range(4)]
                    t_mb = [Tok() for _ in range(NB)]
                    xv = xT.ap().rearrange("(kb p) t -> kb p t", p=128)
                    yv = yout.rearrange("(kb p) t -> kb p t", p=128)
                    b = bank()
                    for kb in range(NB):
                        op("act", lambda e, kb=kb: e.activation(out=psq[:, kb % 2, :], in_=m[:, kb, :], func=AF.Square),
                           reads=[t_m], writes=[t_psq[kb % 2]])
                        op("pe", lambda e, kb=kb, b=b: e.matmul(ps[:, b, :], ones[:], psq[:, kb % 2, :],
                                                               start=(kb == 0), stop=(kb == NB - 1)),
                           reads=[t_psq[kb % 2], t_const], writes=[pst[b]], inc=True)
                    op("act", lambda e, b=b: e.activation(out=prr[:], in_=ps[:, b, :], func=AF.Sqrt, bias=EPS,
                                                          scale=1.0 / D), reads=[pst[b]], writes=[t_prr])
                    op("dve", lambda e: e.reciprocal(out=prr[:], in_=prr[:]), reads=[t_prr], writes=[t_prr])
                    for kb in range(NB):
                        si = kb % 4
                        dma("sp", pxb[:, si, :], xv[kb, :, tsl], reads=[TX[kb]], writes=[t_pxb[si]])
                        op("dve", lambda e, kb=kb: e.scalar_tensor_tensor(
                            out=m[:, kb, :], in0=m[:, kb, :], scalar=gcol(l, gi, kb), in1=prr[:],
                            op0=ALU.mult, op1=ALU.mult), reads=[t_m, t_prr, t_const], writes=[t_mb[kb]])
                        op("dve", lambda e, kb=kb, si=si: e.tensor_tensor(out=m[:, kb, :], in0=m[:, kb, :],
                                                                          in1=pxb[:, si, :], op=ALU.add),
                           reads=[t_pxb[si], t_m], writes=[t_mb[kb]])
                        if final:
                            dma("sp", yv[kb, :, tsl], m[:, kb, :], reads=[t_mb[kb], t_m], writes=[T["yout"]])
                        else:
                            dma("sp", xv[kb, :, tsl], m[:, kb, :], reads=[t_mb[kb], t_m], writes=[TX[kb]])

            def merge_out(l):
              with cx.sbuf("S", [128, NB, NT], BF16) as S:
                t_S = Tok()
                with contextlib.ExitStack() as es:
                    def A(name, shape, dtype):
                        return es.enter_context(cx.sbuf(name, shape, dtype))
                    yb = [A("yb%d" % n, [128, 8, NT], BF16) for n in range(3)]
                    accm = A("accm", [128, 2, NT], F32)
                    sg = A("sg", [128, 2, 512], F32)
                    tmpm = A("tmpm", [128, 2, 512], F32)
                    t_yb = [Tok(), Tok(), Tok()]
                    t_accm = [[Tok(), Tok()] for _ in range(2)]
                    t_sg = [Tok(), Tok()]
                    t_tmp = [Tok(), Tok()]
                    for n, (src, nm) in enumerate(((s_ypool, "ypool"), (s_yconv, "yconv"), (s_yattn, "yattn"))):
                        dma("sp", yb[n][:], src.ap().rearrange("(kb p) t -> p kb t", p=128), reads=[T[nm]], writes=[t_yb[n]])
                    rr = [0]
                    for c in range(8):
                        for n in range(3):
                            def gdst(buf):
                                return buf[:, 0:16 * 256].rearrange("p (kb n) -> p kb n", kb=16)
                            gsrc = w_gate[l].rearrange("(kb p) n -> p kb n", p=128)[:, :, n * D + c * 256:n * D + (c + 1) * 256]

                            def bdst(buf):
                                return buf[:, 16 * 256:24 * 256].rearrange("p (kb n) -> p kb n", kb=8)
                            bsrc = w_branch[l, n].rearrange("(kb p) n -> p kb n", p=128)[:, :, c * 256:(c + 1) * 256]
                            gbuf, gtok = ws.get("gb%d_%d_%d" % (c, n, l), [(gdst, gsrc), (bdst, bsrc)])
                            gv = gbuf[:, 0:16 * 256].rearrange("p (kb n) -> p kb n", kb=16)
                            bv = gbuf[:, 16 * 256:24 * 256].rearrange("p (kb n) -> p kb n", kb=8)
                            btok = gtok
                            for db in range(2):
                                dblk = c * 2 + db
                                for tt in range(2):
                                    tsl = slice(tt * 512, (tt + 1) * 512)
                                    bg = gemm_block(gv, gtok, 16, lambda kb, tsl=tsl: hT[:, kb, tsl], [t_hT[tt]], db * 128)
                                    bu = gemm_block(bv, btok, 8, lambda kb, tsl=tsl, n=n: yb[n][:, kb, tsl], [t_yb[n]], db * 128)
                                    si = rr[0]
                                    rr[0] ^= 1
                                    bc = (l * 3 + n) * NB + dblk
                                    op("act", lambda e, bg=bg, si=si, bc=bc: e.activation(
                                        out=sg[:, si, :], in_=ps[:, bg, :], func=AF.Sigmoid, bias=bgate[:, bc:bc + 1]),
                                       reads=[pst[bg], t_const], writes=[t_sg[si]])
                                    if n == 0:
                                        op("dve", lambda e, bu=bu, si=si, db=db, tsl=tsl: e.tensor_tensor(
                                            out=accm[:, db, tsl], in0=ps[:, bu, :], in1=sg[:, si, :], op=ALU.mult),
                                           reads=[pst[bu], t_sg[si]], writes=[t_accm[db][tt]])
                                    else:
                                        op("dve", lambda e, bu=bu, si=si: e.tensor_tensor(
                                            out=tmpm[:, si, :], in0=ps[:, bu, :], in1=sg[:, si, :], op=ALU.mult),
                                           reads=[pst[bu], t_sg[si]], writes=[t_tmp[si]])
                                        if n == 1:
                                            op("dve", lambda e, si=si, db=db, tsl=tsl: e.tensor_tensor(
                                                out=accm[:, db, tsl], in0=accm[:, db, tsl], in1=tmpm[:, si, :], op=ALU.add),
                                               reads=[t_tmp[si], t_accm[db][tt]], writes=[t_accm[db][tt]])
                                        else:
                                            op("dve", lambda e, si=si, db=db, tsl=tsl, dblk=dblk: e.tensor_tensor(
                                                out=S[:, dblk, tsl], in0=accm[:, db, tsl], in1=tmpm[:, si, :], op=ALU.add),
                                               reads=[t_tmp[si], t_accm[db][tt]], writes=[t_S])
                    cx.barrier()
                with cx.sbuf("mm", [128, NB, 512], F32) as mm:
                    t_mm = Tok()
                    for tt in range(2):
                        tsl = slice(tt * 512, (tt + 1) * 512)
                        for c in range(4):
                            def odst(buf):
                                return buf[:, :].rearrange("p (kb n) -> p kb n", kb=16)
                            osrc = w_out[l].rearrange("(kb p) n -> p kb n", p=128)[:, :, c * 512:(c + 1) * 512]
                            obuf, otok = ws.get("o%d_%d_%d" % (c, tt, l), [(odst, osrc)])
                            ov = obuf[:, :].rearrange("p (kb n) -> p kb n", kb=16)
                            for db in range(4):
                                dblk = c * 4 + db
                                b = gemm_block(ov, otok, 16, lambda kb: S[:, kb, tsl], [t_S], db * 128)
                                op("act", lambda e, b=b, dblk=dblk: e.activation(
                                    out=mm[:, dblk, :], in_=ps[:, b, :], func=AF.Copy),
                                   reads=[pst[b]], writes=[t_mm])
                        post_norm_residual(l, 1, mm, t_mm, tt, False)
                cx.barrier()

            def ffn(l, final):
              with cx.sbuf("actT", [128, 64, 512], BF16) as actT, \
                      cx.sbuf("fr", [128, 2, 512], F32) as fr, \
                      cx.sbuf("fm", [128, NB, 512], F32) as fm:
                t_act = Tok()
                t_fr = [Tok(), Tok()]
                t_fm = Tok()
                for tt in range(2):
                    tsl = slice(tt * 512, (tt + 1) * 512)
                    if True:
                        rr = 0
                        for c in range(16):
                            def udst(buf):
                                return buf[:, :].rearrange("p (kb n) -> p kb n", kb=16)
                            usrc = w_up[l].rearrange("(kb p) n -> p kb n", p=128)[:, :, c * 512:(c + 1) * 512]
                            ubuf, utok = ws.get("u%d_%d_%d" % (c, tt, l), [(udst, usrc)])
                            uv = ubuf[:, :].rearrange("p (kb n) -> p kb n", kb=16)
                            for hb in range(4):
                                b = gemm_block(uv, utok, 16, lambda kb: hT[:, kb, tsl], [t_hT[tt]], hb * 128)
                                si = rr
                                rr ^= 1
                                op("act", lambda e, b=b, si=si: e.activation(out=fr[:, si, :], in_=ps[:, b, :], func=AF.Relu),
                                   reads=[pst[b]], writes=[t_fr[si]])
                                hidx = c * 4 + hb
                                op("dve", lambda e, si=si, hidx=hidx: e.tensor_tensor(
                                    out=actT[:, hidx, :], in0=fr[:, si, :], in1=fr[:, si, :], op=ALU.mult),
                                   reads=[t_fr[si]], writes=[t_act])
                        for c in range(4):
                            banks = []
                            for db in range(4):
                                b = bank()
                                banks.append(b)
                            for hq in range(4):
                                def ddst(buf):
                                    return buf[:, :].rearrange("p (kb n) -> p kb n", kb=16)
                                dsrc = w_down[l].rearrange("(kb p) n -> p kb n", p=128)[:, hq * 16:(hq + 1) * 16, c * 512:(c + 1) * 512]
                                dbuf, dtok = ws.get("d%d_%d_%d_%d" % (c, hq, tt, l), [(ddst, dsrc)])
                                dv = dbuf[:, :].rearrange("p (kb n) -> p kb n", kb=16)
                                for db in range(4):
                                    b = banks[db]
                                    for kb in range(16):
                                        first = (hq == 0 and kb == 0)
                                        last = (hq == 3 and kb == 15)
                                        op("pe", lambda e, b=b, kb=kb, db=db, hq=hq, first=first, last=last: e.matmul(
                                            ps[:, b, :], dv[:, kb, db * 128:(db + 1) * 128], actT[:, hq * 16 + kb, :],
                                            start=first, stop=last),
                                           reads=[dtok, t_act], writes=[pst[b]], inc=(kb == 15))
                            for db in range(4):
                                b = banks[db]
                                dblk = c * 4 + db
                                op("act", lambda e, b=b, dblk=dblk: e.activation(out=fm[:, dblk, :], in_=ps[:, b, :], func=AF.Copy),
                                   reads=[pst[b]], writes=[t_fm])
                        post_norm_residual(l, 3, fm, t_fm, tt, final)
                cx.barrier()

            def scope(name):
                if cx.plan:
                    return contextlib.nullcontext()
                return nc.named_scope(name)
            for l in range(L):
                with scope("norm1"):
                    norm_to_h(l, 0)
                with scope("in_proj"):
                    in_proj(l)
                with scope("attention"):
                    attention(l)
                with scope("merge_out"):
                    merge_out(l)
                with scope("norm2"):
                    norm_to_h(l, 2)
                with scope("ffn"):
                    ffn(l, final=(l == L - 1))
            cx.barrier(engines=("sp", "act", "dve", "pe", "pool"))

    ws = WStream(cx, 3, 16 * 512)
    cx.plan = True
    emit_all()
    cx.plan = False
    cx.setup()
    ws.reset()
    with cx.stack:
        emit_all()
    return nc


def _t5_bucket(n):
    n = np.maximum(n, 0)
    nf = np.maximum(n, 1).astype(np.float32)
    large = 16 + (np.log(nf / 16) / math.log(128 / 16) * 16).astype(np.int32)
    large = np.minimum(large, 31)
    return np.where(n < 16, n, large)


def _core_consts(rank):
    mine = CHUNKS[rank]
    qabs = np.concatenate([np.arange(c * 256, (c + 1) * 256) for c in mine])
    qpos = qabs.reshape(8, 128).T.astype(np.float32).copy()
    kabs = np.concatenate([np.arange(CHUNKS[r][j] * 256, (CHUNKS[r][j] + 1) * 256)
                           for j in range(4) for r in range(2)])
    kpos = np.broadcast_to(kabs.astype(np.uint16), (128, SEQ)).copy()
    sel = np.zeros((8, 16, 2), np.float32)
    for qb in range(8):
        aq = qabs[qb * 128] // 128
        for kb in range(16):
            ak = kabs[kb * 128] // 128
            if aq - ak == 0:
                sel[qb, kb, 0] = 1.0
            elif aq - ak == 1:
                sel[qb, kb, 1] = 1.0
    sel = np.broadcast_to(sel.reshape(1, -1), (128, 256)).copy()
    selh = np.zeros((4, 4, 16), np.float32)
    for j in range(4):
        prev = mine[j] - 1
        for ci, (r, dj) in enumerate(((0, -1), (1, 0), (1, -1), (0, 0))):
            jj = j + dj
            if 0 <= jj < 4 and prev >= 0 and CHUNKS[r][jj] == prev:
                selh[ci, j, :] = 1.0
    selh = np.broadcast_to(selh.reshape(1, -1), (128, 256)).copy()
    invc = np.zeros((4, 4, 16), np.float32)
    for g, w in enumerate(WINS):
        for j in range(4):
            t = mine[j] * 256 + np.arange(16)
            invc[g, j] = 1.0 / np.minimum(t + 1, w)
    invc = np.broadcast_to(invc.reshape(1, -1), (128, 256)).copy()
    return qpos, kpos, sel, selh, invc


def _bias_tables(rel_bias):
    s = np.arange(128)[:, None]
    t = np.arange(128)[None, :]
    out = np.zeros((128, 3, 8, 128), np.float32)
    for a, R in enumerate((0, 128)):
        bk = _t5_bucket(R + t - s)
        out[:, a] = np.transpose(rel_bias[bk], (0, 2, 1))
    out[:, 2] = rel_bias[31][None, :, None]
    return out.reshape(128, -1).copy()


def _per_partition(v, L):
    v = np.asarray(v, np.float32)
    lead = v.shape[:-1]
    nblk = v.shape[-1] // 128
    a = v.reshape(lead + (nblk, 128))
    a = np.moveaxis(a, -1, 0)
    return np.ascontiguousarray(a.reshape(128, -1))


_NC_CACHE = {}


def _get_nc(L):
    if L not in _NC_CACHE:
        _NC_CACHE[L] = build(L)
    return _NC_CACHE[L]


def _run(L, xT_cores, layer0, p):
    nc = _get_nc(L)
    sl = slice(layer0, layer0 + L)
    in_maps = []
    tb = _bias_tables(np.asarray(p["rel_bias"], np.float32))
    ident = np.eye(128, dtype=np.float32).astype(ml_dtypes.bfloat16)
    shared = {
        "w_in": np.ascontiguousarray(p["w_in"][sl]), "pool_w": np.ascontiguousarray(p["pool_w"][sl]),
        "w_branch": np.ascontiguousarray(p["w_branch"][sl]), "w_gate": np.ascontiguousarray(p["w_gate"][sl]),
        "w_out": np.ascontiguousarray(p["w_out"][sl]), "w_up": np.ascontiguousarray(p["w_up"][sl]),
        "w_down": np.ascontiguousarray(p["w_down"][sl]),
        "gains": _per_partition(p["norm_gains"][sl], L), "pscale": _per_partition(p["pool_scale"][sl], L),
        "convw": _per_partition(p["conv_w"][sl], L), "bgate": _per_partition(p["b_gate"][sl], L),
        "tb": tb, "ident": ident,
    }
    for c in range(8):
        qpos, kpos, sel, selh, invc = _core_consts(c % 2)
        m = dict(shared)
        m.update({"xT": xT_cores[c], "qpos": qpos, "kpos": kpos, "sel": sel, "selh": selh, "invc": invc})
        in_maps.append(m)
    res = run_bass_kernel_spmd(nc, in_maps, core_ids=list(range(8)))
    return [np.asarray(r["yT"], np.float32) for r in res.results]


FUSED_LAYERS = 4


def kernel(x, norm_gains, w_in, pool_w, pool_scale, conv_w, rel_bias, w_branch, w_gate, b_gate,
           w_out, w_up, w_down):
    p = dict(norm_gains=np.asarray(norm_gains, np.float32), w_in=np.asarray(w_in, np.float32),
             pool_w=np.asarray(pool_w, np.float32), pool_scale=np.asarray(pool_scale, np.float32),
             conv_w=np.asarray(conv_w, np.float32), rel_bias=np.asarray(rel_bias, np.float32),
             w_branch=np.asarray(w_branch, np.float32), w_gate=np.asarray(w_gate, np.float32),
             b_gate=np.asarray(b_gate, np.float32), w_out=np.asarray(w_out, np.float32),
             w_up=np.asarray(w_up, np.float32), w_down=np.asarray(w_down, np.float32))
    x = np.asarray(x, np.float32)
    depth = p["w_in"].shape[0]
    xT = []
    for c in range(8):
        b, r = c // 2, c % 2
        toks = np.concatenate([x[b, ch * 256:(ch + 1) * 256] for ch in CHUNKS[r]], axis=0)
        xT.append(np.ascontiguousarray(toks.T))
    l0 = 0
    while l0 < depth:
        n = min(FUSED_LAYERS, depth - l0)
        xT = _run(n, xT, l0, p)
        l0 += n
    out = np.zeros_like(x)
    for c in range(8):
        b, r = c // 2, c % 2
        toks = xT[c].T
        for j, ch in enumerate(CHUNKS[r]):
            out[b, ch * 256:(ch + 1) * 256] = toks[j * 256:(j + 1) * 256]
    return out
```
